# Optimizing a Trainium2 kernel written in Bass

```python
import math
import jax, jax.numpy as jnp
from jax import lax
import numpy as np

D_MODEL = 2048
BATCH = 2
SEQ = 16384
DEPTH = 1

EPS = 1e-6
F_GROUPS = 4
F_GROUP_DIM = D_MODEL // 8
F_WIDTH = F_GROUPS * F_GROUP_DIM
GLA_HEADS = 4
GLA_DK = D_MODEL // 16
GLA_DV = D_MODEL // 8
GLA_QK = GLA_HEADS * GLA_DK
GLA_V = GLA_HEADS * GLA_DV
GATE_RANK = 16
GATE_TAU = 16.0
GLA_CHUNK = 64
N_DIRS = 2
W_IN_COLS = F_WIDTH + 2 * GLA_QK + 2 * GLA_V + N_DIRS * GATE_RANK + 2 * D_MODEL
PEER_HEADS = 8
PEER_QDIM = 256
PEER_HALF = PEER_QDIM // 2
N_KEYS = 128
N_EXPERTS = N_KEYS * N_KEYS
PEER_TOPK = 16
PEER_TOKEN_BLOCK = 128

kernel_name = "hybrid_fnet_gla_peer_adaln_block"


def rms_norm(x, g):
    xf = x.astype(jnp.float32)
    y = xf * lax.rsqrt(jnp.mean(xf * xf, axis=-1, keepdims=True) + EPS)
    return (y * g.astype(jnp.float32)).astype(x.dtype)


def fourier_mix(z):
    B, S, _ = z.shape
    zf = z.reshape(B, S, F_GROUPS, F_GROUP_DIM).astype(jnp.float32)
    y = jnp.fft.fftn(zf, axes=(1, 3), norm="ortho").real
    return y.reshape(B, S, F_WIDTH).astype(z.dtype)


def gla_chunked(q, k, v, log_a):
    N, H, S, DK = q.shape
    DV = v.shape[-1]
    nc = S // GLA_CHUNK

    def to_chunks(t):
        t = t.reshape(N, H, nc, GLA_CHUNK, t.shape[-1])
        return jnp.moveaxis(t, 2, 0)

    mask = jnp.tril(jnp.ones((GLA_CHUNK, GLA_CHUNK), dtype=bool))[None, None, :, :, None]

    def step(state, inp):
        qc, kc, vc, ac = inp
        b = jnp.cumsum(ac, axis=-2)
        o_inter = jnp.einsum('nhcd,nhde->nhce', qc * jnp.exp(b), state)
        diff = b[:, :, :, None, :] - b[:, :, None, :, :]
        decay = jnp.exp(jnp.where(mask, diff, -jnp.inf))
        attn = jnp.einsum('nhid,nhjd,nhijd->nhij', qc, kc, decay)
        o_intra = jnp.einsum('nhij,nhje->nhie', attn, vc)
        b_last = b[:, :, -1:, :]
        k_dec = kc * jnp.exp(b_last - b)
        state = jnp.exp(b_last[:, :, 0, :])[..., None] * state + jnp.einsum('nhcd,nhce->nhde', k_dec, vc)
        return state, o_inter + o_intra

    state0 = jnp.zeros((N, H, DK, DV), jnp.float32)
    _, out = lax.scan(step, state0, (to_chunks(q), to_chunks(k), to_chunks(v), to_chunks(log_a)))
    return jnp.moveaxis(out, 0, 2).reshape(N, H, S, DV)


def gla_branch(q, k, v, r, a1, w_a2, b_a, norm_g):
    B, S, _ = q.shape
    dtype = q.dtype
    qh = q.reshape(B, S, GLA_HEADS, GLA_DK).astype(jnp.float32) * (GLA_DK ** -0.5)
    kh = k.reshape(B, S, GLA_HEADS, GLA_DK).astype(jnp.float32)
    vh = v.reshape(B, S, GLA_HEADS, GLA_DV).astype(jnp.float32)
    a1 = a1.reshape(B, S, N_DIRS, GATE_RANK)
    z = jnp.einsum('bsir,ire->ibse', a1, w_a2) + b_a[:, None, None, :]
    log_a = (jax.nn.log_sigmoid(z.astype(jnp.float32)) / GATE_TAU).reshape(N_DIRS, B, S, GLA_HEADS, GLA_DK)

    def both_dirs(t):
        t2 = jnp.stack([t, jnp.flip(t, axis=1)])
        return jnp.transpose(t2.reshape((N_DIRS * B,) + t.shape[1:]), (0, 2, 1, 3))

    la = jnp.stack([log_a[0], jnp.flip(log_a[1], axis=1)])
    la = jnp.transpose(la.reshape(N_DIRS * B, S, GLA_HEADS, GLA_DK), (0, 2, 1, 3))
    o = gla_chunked(both_dirs(qh), both_dirs(kh), both_dirs(vh), la)
    o = o.reshape(N_DIRS, B, GLA_HEADS, S, GLA_DV)
    o = o[0] + jnp.flip(o[1], axis=2)
    o = jnp.transpose(o, (0, 2, 1, 3))
    o = o * lax.rsqrt(jnp.mean(o * o, axis=-1, keepdims=True) + EPS) * norm_g.astype(jnp.float32)
    o = o * jax.nn.silu(r.reshape(B, S, GLA_HEADS, GLA_DV).astype(jnp.float32))
    return o.reshape(B, S, GLA_V).astype(dtype)


def peer_ffn(n2, w_q, keys, u, v):
    B, S, D = n2.shape
    q = (n2 @ w_q).reshape(B, S, PEER_HEADS, PEER_QDIM).astype(jnp.float32)
    s1 = jnp.einsum('bshd,hnd->bshn', q[..., :PEER_HALF], keys[:, 0].astype(jnp.float32))
    s2 = jnp.einsum('bshd,hnd->bshn', q[..., PEER_HALF:], keys[:, 1].astype(jnp.float32))
    v1, i1 = lax.top_k(s1, PEER_TOPK)
    v2, i2 = lax.top_k(s2, PEER_TOPK)
    cand = (v1[..., :, None] + v2[..., None, :]).reshape(B, S, PEER_HEADS, PEER_TOPK * PEER_TOPK)
    sv, pos = lax.top_k(cand, PEER_TOPK)
    e1 = jnp.take_along_axis(i1, pos // PEER_TOPK, axis=-1)
    e2 = jnp.take_along_axis(i2, pos % PEER_TOPK, axis=-1)
    idx = e1 * N_KEYS + e2
    g = jax.nn.softmax(sv, axis=-1)

    T = B * S
    nb = T // PEER_TOKEN_BLOCK
    xb = n2.reshape(nb, PEER_TOKEN_BLOCK, D)
    ib = idx.reshape(nb, PEER_TOKEN_BLOCK, PEER_HEADS * PEER_TOPK)
    gb = g.reshape(nb, PEER_TOKEN_BLOCK, PEER_HEADS * PEER_TOPK)

    def block(args):
        xt, it, gt = args
        U = jnp.take(u, it, axis=0)
        act = jax.nn.gelu(jnp.einsum('td,tkd->tk', xt, U).astype(jnp.float32), approximate=False)
        V = jnp.take(v, it, axis=0)
        return jnp.einsum('tk,tkd->td', (gt * act).astype(xt.dtype), V)

    out = lax.map(block, (xb, ib, gb))
    return out.reshape(B, S, D)


def setup_inputs(seed: int = 0) -> dict:
    key = jax.random.key(seed)
    ks = jax.random.split(key, 20)
    D = D_MODEL
    f32 = jnp.float32
    nrm = lambda k, shape, s: jax.random.normal(k, shape, f32) * s
    return {
        "x": nrm(ks[0], (BATCH, SEQ, D), 1.0),
        "c": nrm(ks[1], (BATCH, D), 1.0),
        "w_ada": nrm(ks[2], (DEPTH, D, 6 * D), D ** -0.5),
        "b_ada": nrm(ks[3], (DEPTH, 6 * D), 0.02),
        "norm1_g": 1.0 + nrm(ks[4], (DEPTH, D), 0.02),
        "w_in": nrm(ks[5], (DEPTH, D, W_IN_COLS), D ** -0.5),
        "w_fnet": nrm(ks[6], (DEPTH, F_WIDTH, D), F_WIDTH ** -0.5),
        "gla_w_a2": nrm(ks[7], (DEPTH, N_DIRS, GATE_RANK, GLA_QK), GATE_RANK ** -0.5),
        "gla_b_a": nrm(ks[8], (DEPTH, N_DIRS, GLA_QK), 0.1),
        "gla_norm_g": 1.0 + nrm(ks[9], (DEPTH, GLA_DV), 0.02),
        "w_gla": nrm(ks[10], (DEPTH, GLA_V, D), GLA_V ** -0.5),
        "w_out": nrm(ks[11], (DEPTH, D, D), D ** -0.5),
        "norm2_g": 1.0 + nrm(ks[12], (DEPTH, D), 0.02),
        "peer_w_q": nrm(ks[13], (DEPTH, D, PEER_HEADS * PEER_QDIM), D ** -0.5),
        "peer_keys": nrm(ks[14], (DEPTH, PEER_HEADS, 2, N_KEYS, PEER_HALF), PEER_HALF ** -0.5),
        "peer_u": nrm(ks[15], (DEPTH, N_EXPERTS, D), D ** -0.5),
        "peer_v": nrm(ks[16], (DEPTH, N_EXPERTS, D), 0.5),
        "final_norm_g": 1.0 + nrm(ks[17], (D,), 0.02),
    }


def reference(x, c, w_ada, b_ada, norm1_g, w_in, w_fnet, gla_w_a2, gla_b_a, gla_norm_g,
              w_gla, w_out, norm2_g, peer_w_q, peer_keys, peer_u, peer_v, final_norm_g):
    split_points = [F_WIDTH,
                    F_WIDTH + GLA_QK,
                    F_WIDTH + 2 * GLA_QK,
                    F_WIDTH + 2 * GLA_QK + GLA_V,
                    F_WIDTH + 2 * GLA_QK + 2 * GLA_V,
                    F_WIDTH + 2 * GLA_QK + 2 * GLA_V + N_DIRS * GATE_RANK]
    h = x
    for l in range(DEPTH):
        mod = jax.nn.silu(c) @ w_ada[l] + b_ada[l]
        sh1, sc1, g1, sh2, sc2, g2 = jnp.split(mod[:, None, :], 6, axis=-1)

        n = rms_norm(h, norm1_g[l]) * (1.0 + sc1) + sh1
        p = n @ w_in[l]
        z_f, q, k, v, r, a1, m = jnp.split(p, split_points, axis=-1)
        y_f = fourier_mix(z_f) @ w_fnet[l]
        y_g = gla_branch(q, k, v, r, a1, gla_w_a2[l], gla_b_a[l], gla_norm_g[l]) @ w_gla[l]
        gate_f, gate_g = jnp.split(jax.nn.sigmoid(m), 2, axis=-1)
        mixed = (gate_f * y_f + gate_g * y_g) @ w_out[l]
        h = h + g1 * mixed

        n2 = rms_norm(h, norm2_g[l]) * (1.0 + sc2) + sh2
        h = h + g2 * peer_ffn(n2, peer_w_q[l], peer_keys[l], peer_u[l], peer_v[l])
    return rms_norm(h, final_norm_g)
```

```python
import math
from contextlib import ExitStack
import numpy as np
import ml_dtypes
import concourse.bass as bass
import concourse.mybir as mybir
from concourse.bass_utils import run_bass_kernel_spmd

F32 = mybir.dt.float32
BF16 = mybir.dt.bfloat16
U32 = mybir.dt.uint32
I32 = mybir.dt.int32
AF = mybir.ActivationFunctionType
ALU = mybir.AluOpType
AX = mybir.AxisListType

D = 2048
S = 16384
TOWN = 4096
NCORES = 8
EPS = 1e-6
SEM_CAP = 30000


class Buf:
    def __init__(self, ap):
        self.ap = ap
        self.w = {}
        self.r = {}
        self.dsem = None
        self.dcnt = 0

    def __getitem__(self, k):
        return V(self, self.ap[k])

    def v(self):
        return V(self, self.ap)


class V:
    def __init__(self, buf, ap, extra=()):
        self.buf = buf
        self.ap = ap
        self.extra = tuple(extra)

    def rr(self, s, **kw):
        return V(self.buf, self.ap.rearrange(s, **kw), self.extra)

    def bc(self, shape):
        return V(self.buf, self.ap.broadcast_to(shape), self.extra)

    def us(self, ax):
        return V(self.buf, self.ap.unsqueeze(ax), self.extra)

    def bitcast(self, dt):
        return V(self.buf, self.ap.bitcast(dt), self.extra)

    def __getitem__(self, k):
        return V(self.buf, self.ap[k], self.extra)

    def bufs(self):
        return (self.buf,) + self.extra


class KB:
    def __init__(self, nc, es):
        self.nc = nc
        self.es = es
        self.eng = {"pe": nc.tensor, "act": nc.scalar, "dve": nc.vector, "pool": nc.gpsimd, "sp": nc.sync}
        self.esem = {}
        self.ecnt = {}
        self.waited = {e: {} for e in self.eng}
        self.nsem = 0
        self.allsems = {}
        for e in ("pe", "act", "dve", "pool"):
            self._newsem(e)
        self.ninst = 0

    def sem(self):
        self.nsem += 1
        s = self.es.enter_context(self.nc.semaphore("s%d" % self.nsem))
        self.allsems[s] = 0
        return s

    def _newsem(self, e):
        self.esem[e] = self.sem()
        self.ecnt[e] = 0

    def _wait(self, e, evs):
        w = self.waited[e]
        for sem, val in evs.items():
            if w.get(sem, 0) >= val:
                continue
            self.eng[e].wait_ge(sem, val)
            w[sem] = val

    def _collect(self, e, reads, writes):
        evs = {}
        own = self.esem.get(e)
        for v in reads:
            for b in v.bufs():
                for sem, val in b.w.items():
                    if e == "pe" and sem is own:
                        continue
                    evs[sem] = max(evs.get(sem, 0), val)
        for v in writes:
            for b in v.bufs():
                for sem, val in list(b.w.items()) + list(b.r.items()):
                    if sem is own:
                        continue
                    evs[sem] = max(evs.get(sem, 0), val)
        return evs

    def op(self, e, reads, writes, fn):
        reads = [v for v in reads if isinstance(v, V)]
        self._wait(e, self._collect(e, reads, writes))
        if self.ecnt[e] >= SEM_CAP:
            self._newsem(e)
        inst = fn(self.eng[e])
        self.ecnt[e] += 1
        sem = self.esem[e]
        inst.then_inc(sem, 1)
        self.allsems[sem] = self.ecnt[e]
        for v in reads:
            for b in v.bufs():
                b.r[sem] = self.ecnt[e]
        for v in writes:
            for b in v.bufs():
                b.w[sem] = self.ecnt[e]
        self.ninst += 1

    def dma(self, out, in_, q="sp", **kw):
        self._wait(q, self._collect(q, [in_], [out]))
        b = out.buf
        if b.dsem is None or b.dcnt >= SEM_CAP:
            b.dsem = self.sem()
            b.dcnt = 0
        inst = self.eng[q].dma_start(out=out.ap, in_=in_.ap, **kw)
        b.dcnt += 16
        inst.then_inc(b.dsem, 16)
        self.allsems[b.dsem] = b.dcnt
        for bb in out.bufs():
            bb.w[b.dsem] = b.dcnt
        for bb in in_.bufs():
            bb.r[b.dsem] = b.dcnt
        self.ninst += 1

    def barrier(self):
        evs = dict((s, v) for s, v in self.allsems.items() if v > 0)
        for e in self.eng:
            own = self.esem.get(e)
            self._wait(e, dict((s, v) for s, v in evs.items() if s is not own))

    def mm(self, out, lhsT, rhs, start=True, stop=True):
        self.op("pe", [lhsT, rhs], [out], lambda g: g.matmul(out.ap, lhsT=lhsT.ap, rhs=rhs.ap, start=start, stop=stop))

    def tr(self, out, in_, ident):
        self.op("pe", [in_, ident], [out], lambda g: g.transpose(out.ap, in_.ap, ident.ap))

    def act(self, out, in_, func, bias=None, scale=None, accum=None, e="act"):
        kw = {}
        rd = [in_]
        if bias is not None:
            kw["bias"] = bias.ap if isinstance(bias, V) else bias
            rd.append(bias)
        if scale is not None:
            kw["scale"] = scale.ap if isinstance(scale, V) else scale
            rd.append(scale)
        wr = [out]
        if accum is not None:
            kw["accum_out"] = accum.ap
            wr.append(accum)
        self.op("act", rd, wr, lambda g: g.activation(out=out.ap, in_=in_.ap, func=func, **kw))

    def tt(self, e, out, in0, in1, op):
        self.op(e, [in0, in1], [out], lambda g: g.tensor_tensor(out=out.ap, in0=in0.ap, in1=in1.ap, op=op))

    def ts(self, e, out, in0, s1, s2=None, op0=ALU.mult, op1=None, accum=None):
        rd = [in0, s1, s2]
        a1 = s1.ap if isinstance(s1, V) else s1
        a2 = s2.ap if isinstance(s2, V) else s2
        kw = {}
        if op1 is not None:
            kw["op1"] = op1
        wr = [out]
        if accum is not None:
            kw["accum_out"] = accum.ap
            wr.append(accum)
        self.op(e, rd, wr, lambda g: g.tensor_scalar(out=out.ap, in0=in0.ap, scalar1=a1, scalar2=a2, op0=op0, **kw))

    def stt(self, e, out, in0, scalar, in1, op0, op1):
        sc = scalar.ap if isinstance(scalar, V) else scalar
        self.op(e, [in0, scalar, in1], [out], lambda g: g.scalar_tensor_tensor(out=out.ap, in0=in0.ap, scalar=sc, in1=in1.ap, op0=op0, op1=op1))

    def cp(self, e, out, in_):
        if e == "act":
            self.op(e, [in_], [out], lambda g: g.copy(out=out.ap, in_=in_.ap))
        else:
            self.op(e, [in_], [out], lambda g: g.tensor_copy(out=out.ap, in_=in_.ap))

    def recip(self, out, in_):
        self.op("dve", [in_], [out], lambda g: g.reciprocal(out=out.ap, in_=in_.ap))

    def memset(self, e, out, val):
        self.op(e, [], [out], lambda g: g.memset(out.ap, val))


class Phase:
    def __init__(self, kb, name):
        self.kb = kb
        self.name = name
        self.es = ExitStack()
        self.n = 0

    def __enter__(self):
        self.es.__enter__()
        return self

    def __exit__(self, *a):
        self.kb.barrier()
        return self.es.__exit__(*a)

    def sb(self, shape, dt, name=None):
        self.n += 1
        t = self.es.enter_context(self.kb.nc.sbuf_tensor("%s_%s%d" % (self.name, name or "t", self.n), list(shape), dt))
        return Buf(t[:])

    def ps(self, shape, dt, name=None):
        self.n += 1
        t = self.es.enter_context(self.kb.nc.psum_tensor("%s_%s%d" % (self.name, name or "p", self.n), list(shape), dt))
        return Buf(t[:])


def host_consts(r):
    c = {}
    c["identf"] = np.eye(128, dtype=np.float32)
    c["identb"] = np.eye(128).astype(ml_dtypes.bfloat16)
    t = np.arange(64)
    fw = (t[:, None] <= t[None, :]).astype(np.float32)
    bw = (t[:, None] >= t[None, :]).astype(np.float32)
    tri = np.zeros((64, 2, 3, 64), np.float32)
    for d, m in enumerate((fw, bw)):
        tri[:, d, 0, :] = -m / 16.0
        tri[:, d, 1, :] = -(1.0 - m) / 16.0
        tri[:, d, 2, :] = m
    c["tri"] = tri.reshape(64, 384)
    ih = np.arange(128)
    ang = 2 * np.pi * np.outer(ih, ih) / 128.0
    c["t1"] = (np.concatenate([np.cos(ang), -np.sin(ang)], axis=1) / 128.0).astype(ml_dtypes.bfloat16)
    T0 = TOWN * r
    il = (T0 + np.arange(128)).astype(np.int64)
    sp = (T0 + 128 * np.arange(32)[None, :] + np.arange(128)[:, None]).astype(np.int64)
    ph = (il[:, None, None] * sp[None, :, :]) % S
    a2 = 2 * np.pi * ph / S
    ec, es_ = np.cos(a2), np.sin(a2)
    c["e2a"] = np.concatenate([ec, -es_], axis=2).reshape(128, 128 * 64).astype(ml_dtypes.bfloat16)
    c["e2b"] = np.concatenate([es_, ec], axis=2).reshape(128, 128 * 64).astype(ml_dtypes.bfloat16)
    ch = np.arange(256)
    a3 = 2 * np.pi * np.outer(ch, ch) / 256.0
    c["cc"] = (np.concatenate([np.cos(a3), np.sin(a3)], axis=1) / 16.0).astype(ml_dtypes.bfloat16)
    pos = np.arange(256) * 64
    mf = (pos >= S - TOWN * r).astype(np.float32)
    mb = (pos < TOWN + (3 - r) * TOWN).astype(np.float32)
    c["cmask"] = np.tile(((np.concatenate([mf, mb]) - 1.0) * 30000.0)[None, :], (64, 1)).astype(np.float32)
    c["iota"] = np.tile(np.arange(128, dtype=np.float32)[None, :], (128, 1))
    return c


CONST_SPECS = [("identf", [128, 128], F32), ("identb", [128, 128], BF16), ("tri", [64, 384], F32),
               ("t1", [128, 256], BF16), ("e2a", [128, 8192], BF16), ("e2b", [128, 8192], BF16),
               ("cc", [256, 512], BF16), ("cmask", [64, 512], F32), ("iota", [128, 128], F32)]

IN_SPECS = [("xs", [S, D]), ("c", [1, D]), ("w_ada", [D, 6 * D]), ("b_ada", [1, 6 * D]), ("norm1_g", [1, D]),
            ("w_in", [D, 8224]), ("w_fnet", [1024, D]), ("gla_w_a2", [2, 16, 512]), ("gla_b_a", [2, 512]),
            ("gla_norm_g", [1, 256]), ("w_gla", [1024, D]), ("w_out", [D, D]), ("norm2_g", [1, D]),
            ("peer_w_q", [D, D]), ("peer_keys", [16, 128, 128]), ("peer_u", [16384, D]), ("peer_v", [16384, D]),
            ("final_norm_g", [1, D])]


class Prog:
    def __init__(self, stop_after=None, debug=()):
        self.stop_after = stop_after
        self.debug = set(debug)
        self.nc = nc = bass.Bass("TRN2", target_bir_lowering=False)
        self.inp = {}
        for name, shape in IN_SPECS:
            self.inp[name] = Buf(nc.dram_tensor(name, list(shape), F32, kind="ExternalInput").ap())
        for name, shape, dt in CONST_SPECS:
            self.inp[name] = Buf(nc.dram_tensor(name, list(shape), dt, kind="ExternalInput").ap())
        self.inp["x_own"] = Buf(nc.dram_tensor("x_own", [TOWN, D], F32, kind="ExternalInput").ap())
        self.out = Buf(nc.dram_tensor("out", [TOWN, D], F32, kind="ExternalOutput").ap())
        self.scr = {}

    def scratch(self, name, shape, dt):
        kind = "ExternalOutput" if name in self.debug else "Internal"
        b = Buf(self.nc.dram_tensor(name, list(shape), dt, kind=kind).ap())
        self.scr[name] = b
        return b

    def build(self):
        with ExitStack() as es:
            self.kb = kb = KB(self.nc, es)
            with Phase(kb, "glob") as gp:
                self.gp = gp
                self._build(kb, gp)
                kb.barrier()
        return self.nc

    def _build(self, kb, gp):
        I = self.inp
        stages = [self.s_setup, self.s_wconv, self.s_projA, self.s_dft, self.s_gla, self.s_mix, self.s_peer_prep,
                  self.s_peer]
        for st in stages:
            st(kb, gp)
            if self.stop_after == st.__name__:
                with Phase(kb, "fin") as ph:
                    z = ph.sb([128, D], F32)
                    kb.memset("dve", z.v(), 0.0)
                    for i in range(TOWN // 128):
                        kb.dma(self.out[i * 128:(i + 1) * 128, :], z.v())
                return

    def s_setup(self, kb, gp):
        I = self.inp
        self.identf = gp.sb([128, 128], F32, "identf")
        self.identb = gp.sb([128, 128], BF16, "identb")
        self.tri = gp.sb([64, 384], F32, "tri")
        self.iota = gp.sb([128, 128], F32, "iota")
        kb.dma(self.identf.v(), I["identf"].v())
        kb.dma(self.identb.v(), I["identb"].v())
        kb.dma(self.tri.v(), I["tri"].v())
        kb.dma(self.iota.v(), I["iota"].v())
        self.modT = gp.sb([128, 96], F32, "modT")
        self.gam1 = gp.sb([128, 16], F32, "gam1")
        self.gam2 = gp.sb([128, 16], F32, "gam2")
        self.g2b = gp.sb([128, D], F32, "g2b")
        self.fngb = gp.sb([128, D], F32, "fngb")
        self.gngb = gp.sb([64, 256], F32, "gngb")
        kb.dma(self.fngb.v(), I["final_norm_g"].v().bc([128, D]))
        kb.dma(self.gngb.v(), I["gla_norm_g"].v().bc([64, 256]))
        with Phase(kb, "ada") as ph:
            rows = ph.sb([128, 128], F32)
            rowsb = ph.sb([96, 128], F32)
            kb.dma(rows[0:16, :], I["c"].v().rr("o (k p) -> (o k) p", p=128))
            kb.dma(rows[16:32, :], I["norm1_g"].v().rr("o (k p) -> (o k) p", p=128))
            kb.dma(rows[32:48, :], I["norm2_g"].v().rr("o (k p) -> (o k) p", p=128))
            kb.dma(rowsb.v(), I["b_ada"].v().rr("o (k p) -> (o k) p", p=128))
            pst = ph.ps([128, 512], F32)
            kb.mm(pst[:, 0:48], rows[0:48, :], self.identf[0:48, 0:48])
            kb.mm(pst[:, 48:144], rowsb[0:96, :], self.identf[0:96, 0:96])
            colsT = ph.sb([128, 144], F32)
            kb.cp("dve", colsT.v(), pst[:, 0:144])
            silc = ph.sb([128, 16], F32)
            kb.act(silc.v(), colsT[:, 0:16], AF.Silu)
            psm = ph.ps([128, 96], F32)
            wa = [ph.sb([128, 16, 512], F32) for _ in range(2)]
            self.Wb = self.scratch("Wb", [128, 16, 8224], BF16)
            wst = [ph.sb([128, 16, 512], F32) for _ in range(2)]
            wcb = [ph.sb([128, 16, 512], BF16) for _ in range(2)]

            def wconv_step(i):
                c0 = i * 512
                n = min(512, 8224 - c0)
                s_, c_ = wst[i % 2], wcb[i % 2]
                kb.dma(s_[:, :, 0:n], I["w_in"][:, c0:c0 + n].rr("(k p) c -> p k c", p=128))
                kb.cp("dve", c_[:, 0:8, 0:n], s_[:, 0:8, 0:n])
                kb.cp("act", c_[:, 8:16, 0:n], s_[:, 8:16, 0:n])
                kb.dma(self.Wb[:, :, c0:c0 + n], c_[:, :, 0:n], q="pool")
            for blk in range(24):
                w = wa[blk % 2]
                kb.dma(w.v(), I["w_ada"][:, blk * 512:(blk + 1) * 512].rr("(k p) c -> p k c", p=128))
                if blk < 17:
                    wconv_step(blk)
                for j in range(4):
                    for kc in range(16):
                        kb.mm(psm[:, blk * 4 + j:blk * 4 + j + 1], w[:, kc, j * 128:(j + 1) * 128], silc[:, kc:kc + 1],
                              start=(kc == 0), stop=(kc == 15))
            kb.tt("dve", self.modT.v(), psm.v(), colsT[:, 48:144], ALU.add)
            m = self.modT
            self.sh1, self.sc1, self.g1, self.sh2, self.sc2, self.g2 = [m[:, 16 * i:16 * (i + 1)] for i in range(6)]
            kb.stt("dve", self.gam1.v(), self.sc1, 1.0, colsT[:, 16:32], ALU.add, ALU.mult)
            kb.stt("dve", self.gam2.v(), self.sc2, 1.0, colsT[:, 32:48], ALU.add, ALU.mult)
            rep = ph.sb([128, 16, 128], F32)
            pb = ph.ps([128, D], F32)
            for gsrc, gdst in ((self.g2, self.g2b),):
                kb.cp("dve", rep.v(), gsrc.us(2).bc([128, 16, 128]))
                for kc in range(16):
                    kb.mm(pb[:, kc * 128:(kc + 1) * 128], rep[:, kc, :], self.identf.v())
                kb.cp("act", gdst.v(), pb.v())
        if "dbg_mod" in self.debug:
            d = self.scratch("dbg_mod", [128, 96], F32)
            kb.dma(d.v(), self.modT.v())
            d2 = self.scratch("dbg_g1b", [128, D], F32)
            kb.dma(d2.v(), self.g2b.v())

    def s_wconv(self, kb, gp):
        pass

    def norm_transpose(self, kb, ph, xt, nsub, gam, sh, nT, pst, xb, ss, rstd, junk):
        for s in range(nsub):
            kb.act(junk.v() if junk is not None else xb[:, s, :], xt[:, s, :], AF.Square, accum=ss[:, s:s + 1])
        kb.ts("dve", rstd[:, 0:nsub], ss[:, 0:nsub], 1.0 / D, EPS, op0=ALU.mult, op1=ALU.add)
        kb.act(rstd[:, 0:nsub], rstd[:, 0:nsub], AF.Sqrt)
        kb.recip(rstd[:, 0:nsub], rstd[:, 0:nsub])
        for s in range(nsub):
            if s % 2 == 0:
                kb.ts("dve", xb[:, s, :], xt[:, s, :], rstd[:, s:s + 1], None, op0=ALU.mult)
            else:
                kb.act(xb[:, s, :], xt[:, s, :], AF.Identity, scale=rstd[:, s:s + 1])
        for kc in range(16):
            p = pst[kc % 2]
            for s in range(nsub):
                kb.tr(p[:, s * 128:(s + 1) * 128], xb[:, s, kc * 128:(kc + 1) * 128], self.identb.v())
            if kc % 2 == 0:
                kb.act(nT[:, kc, :], p[:, 0:nsub * 128], AF.Identity, bias=sh[:, kc:kc + 1], scale=gam[:, kc:kc + 1])
            else:
                kb.ts("dve", nT[:, kc, :], p[:, 0:nsub * 128], gam[:, kc:kc + 1], sh[:, kc:kc + 1], op0=ALU.mult, op1=ALU.add)

    def s_projA(self, kb, gp):
        I = self.inp
        zf_d = self.scratch("zf_d", [S, 1024], BF16)
        k_d = self.scratch("k_d", [S, 512], BF16)
        v_d = self.scratch("v_d", [S, 1024], BF16)
        a1T_d = self.scratch("a1T_d", [32, S], BF16)
        qT_d = self.scratch("qT_d", [512, TOWN], BF16)
        kT_d = self.scratch("kT_d", [512, TOWN], BF16)
        r_d = self.scratch("r_d", [TOWN, 1024], BF16)
        m_d = self.scratch("m_d", [TOWN, 4096], BF16)
        ctx_tiles = [("zf", 0, 512, zf_d, 0), ("zf", 512, 512, zf_d, 512), ("k", 1536, 512, k_d, 0),
                     ("v", 2048, 512, v_d, 0), ("v", 2560, 512, v_d, 512), ("a", 4096, 32, a1T_d, 0)]
        own_tiles = ctx_tiles + [("q", 1024, 512, qT_d, 0), ("r", 3072, 512, r_d, 0), ("r", 3584, 512, r_d, 512)] + \
            [("m", 4128 + 512 * i, 512, m_d, 512 * i) for i in range(8)]
        with Phase(kb, "projA") as ph:
            xt = [ph.sb([128, 4, D], F32) for _ in range(2)]
            xbs = [ph.sb([128, 4, D], BF16) for _ in range(2)]
            junk = None
            sss = [ph.sb([128, 4], F32) for _ in range(2)]
            rstds = [ph.sb([128, 4], F32) for _ in range(2)]
            nTs = [ph.sb([128, 16, 512], BF16) for _ in range(2)]
            wt = [ph.sb([128, 16, 512], BF16) for _ in range(3)]
            stg = [ph.sb([128, 4, 512], BF16) for _ in range(2)]
            stgf = ph.sb([32, 512], BF16)
            pst = [ph.ps([128, 1024], BF16) for _ in range(2)]
            pso = [ph.ps([128, 512], F32) for _ in range(4)]
            nw = 0
            no = 0
            ns = 0
            for tile in range(32):
                x_ = xt[tile % 2]
                kb.dma(x_.v(), I["xs"][tile * 512:(tile + 1) * 512, :].rr("(s p) d -> p s d", p=128))
                nT, xb, ss, rstd = nTs[tile % 2], xbs[tile % 2], sss[tile % 2], rstds[tile % 2]
                self.norm_transpose(kb, ph, x_, 4, self.gam1, self.sh1, nT, pst, xb, ss, rstd, junk)
                own = tile < 8
                t0 = tile * 512
                for (kind, c0, n, dst, dc0) in (own_tiles if own else ctx_tiles):
                    w = wt[nw % 3]
                    nw += 1
                    kb.dma(w[:, :, 0:n], self.Wb[:, :, c0:c0 + n])
                    if kind in ("zf", "k", "v", "r", "m"):
                        sg = stg[ns % 2]
                        ns += 1
                        for s in range(4):
                            p = pso[no % 4]
                            no += 1
                            for kc in range(16):
                                kb.mm(p.v(), nT[:, kc, s * 128:(s + 1) * 128], w[:, kc, :], start=(kc == 0), stop=(kc == 15))
                            if kind == "r":
                                kb.act(sg[:, s, :], p.v(), AF.Silu)
                            elif kind == "m":
                                kb.act(sg[:, s, :], p.v(), AF.Sigmoid)
                            else:
                                kb.cp("act" if s % 2 == 0 else "dve", sg[:, s, :], p.v())
                        kb.dma(dst[t0:t0 + 512, dc0:dc0 + 512].rr("(s p) c -> p s c", p=128), sg.v(), q="pool")
                    if kind == "a":
                        p = pso[no % 4]
                        no += 1
                        for kc in range(16):
                            kb.mm(p[0:32, :], w[:, kc, 0:32], nT[:, kc, :], start=(kc == 0), stop=(kc == 15))
                        kb.cp("dve", stgf.v(), p[0:32, :])
                        kb.dma(a1T_d[:, t0:t0 + 512], stgf.v(), q="pool")
                    if kind == "q" or (kind == "k" and own):
                        sg = stg[ns % 2]
                        ns += 1
                        for cc in range(4):
                            p = pso[no % 4]
                            no += 1
                            for kc in range(16):
                                kb.mm(p.v(), w[:, kc, cc * 128:(cc + 1) * 128], nT[:, kc, :], start=(kc == 0), stop=(kc == 15))
                            kb.cp("act" if cc % 2 == 0 else "dve", sg[:, cc, :], p.v())
                        dd = qT_d if kind == "q" else kT_d
                        kb.dma(dd[:, t0:t0 + 512].rr("(c p) t -> p c t", p=128), sg.v(), q="pool")

    def s_dft(self, kb, gp):
        I = self.inp
        zf_d = self.scr["zf_d"]
        UT_d = self.scratch("UT_d", [8, 128, 2, TOWN], BF16)
        zv = zf_d.v().rr("(p r) c -> p r c", p=128)
        with Phase(kb, "dft") as ph:
            t1 = ph.sb([128, 256], BF16)
            e2a = ph.sb([128, 128, 64], BF16)
            e2b = ph.sb([128, 128, 64], BF16)
            kb.dma(t1.v(), I["t1"].v())
            kb.dma(e2a.v(), I["e2a"].v().rr("p (k n) -> p k n", n=64))
            kb.dma(e2b.v(), I["e2b"].v().rr("p (k n) -> p k n", n=64))
            zb = [ph.sb([128, 128, 128], BF16) for _ in range(2)]
            A = ph.sb([128, 128, 2, 128], BF16)
            ut = [ph.sb([128, 2, 32, 128], BF16) for _ in range(1)]
            ps1 = [ph.ps([128, 512], F32) for _ in range(3)]
            ps2 = [ph.ps([128, 512], F32) for _ in range(3)]
            n1 = n2 = 0
            for blk in range(8):
                z = zb[blk % 2]
                for qd in range(4):
                    kb.dma(z[:, qd * 32:(qd + 1) * 32, :], zv[:, qd * 32:(qd + 1) * 32, blk * 128:(blk + 1) * 128])
                for cp_ in range(64):
                    p = ps1[n1 % 3]
                    n1 += 1
                    for j in range(2):
                        ch = cp_ * 2 + j
                        kb.mm(p[:, j * 256:(j + 1) * 256], z[:, :, ch], t1.v())
                    kb.cp("act" if cp_ % 2 == 0 else "dve", A[:, cp_ * 2:cp_ * 2 + 2, :, :].rr("p a b c -> p (a b c)"), p.v())
                u = ut[0]
                for kg in range(16):
                    p = ps2[n2 % 3]
                    n2 += 1
                    for j in range(8):
                        kl = kg * 8 + j
                        kb.mm(p[:, j * 64:(j + 1) * 64], A[:, :, 0, kl], e2a[:, kl, :], start=True, stop=False)
                        kb.mm(p[:, j * 64:(j + 1) * 64], A[:, :, 1, kl], e2b[:, kl, :], start=False, stop=True)
                    kb.cp("act" if kg % 2 == 0 else "dve", u[:, :, :, kg * 8:(kg + 1) * 8],
                          p.v().rr("p (k c h) -> p c h k", k=8, c=2, h=32))
                kb.dma(UT_d[blk].rr("p c t -> p (c t)"), u.v().rr("p c h k -> p (c h k)"), q="pool")

    def s_gla(self, kb, gp):
        I = self.inp
        S_ = self.scr
        k_d, v_d, a1T_d, qT_d, kT_d, r_d = S_["k_d"], S_["v_d"], S_["a1T_d"], S_["qT_d"], S_["kT_d"], S_["r_d"]
        o_d = self.scratch("o_d", [TOWN, 1024], F32)
        og_d = self.scratch("og_d", [TOWN, 1024], BF16)
        NB = 4
        BT = NB * 64
        with Phase(kb, "gla") as ph:
            wabf = [ph.sb([17, 512], F32) for _ in range(2)]
            wab = [ph.sb([17, 512], BF16) for _ in range(2)]
            for d in range(2):
                kb.dma(wabf[d][0:16, :], I["gla_w_a2"][d])
                kb.dma(wabf[d][16:17, :], I["gla_b_a"][d:d + 1, :])
                kb.cp("dve", wab[d].v(), wabf[d].v())
            tri16 = ph.sb([64, 384], BF16)
            kb.cp("dve", tri16.v(), self.tri.v())
            cm = ph.sb([64, 512], F32)
            kb.dma(cm.v(), I["cmask"].v())
            negcol = ph.sb([64, 1], BF16)
            kb.memset("dve", negcol.v(), -1.0 / 16.0)
            St = [ph.sb([128, 256], F32) for _ in range(4)]
            Sb = [ph.sb([128, 256], BF16) for _ in range(4)]
            kblk = [ph.sb([64, NB, 512], BF16) for _ in range(2)]
            vblk = [ph.sb([64, NB, 1024], BF16) for _ in range(2)]
            a16 = [ph.sb([17, BT], BF16) for _ in range(2)]
            for b_ in a16:
                kb.memset("dve", b_.v(), 1.0)
            sp16 = [ph.sb([64, 512], BF16) for _ in range(3)]
            splo = [ph.sb([64, 512], BF16) for _ in range(3)]
            qblk = [ph.sb([128, 4, BT], BF16) for _ in range(2)]
            ktblk = [ph.sb([128, 4, BT], BF16) for _ in range(2)]
            oblk = [ph.sb([64, NB, 1024], F32) for _ in range(2)]
            rblk = [ph.sb([64, NB, 1024], BF16) for _ in range(2)]
            ogstg = [ph.sb([64, NB, 1024], BF16) for _ in range(2)]
            ez = ph.sb([64, 512], F32)
            sp = [ph.sb([64, 512], F32) for _ in range(3)]
            ed = ph.sb([64, 512], F32)
            kd = [ph.sb([64, 512], BF16) for _ in range(3)]
            ebl = [ph.sb([128, 4], F32) for _ in range(3)]
            eb = ph.sb([128, 4, 64], F32)
            enb = ph.sb([128, 4, 64], F32)
            qtl = [ph.sb([128, 4, 64], BF16) for _ in range(3)]
            ktl = [ph.sb([128, 4, 64], BF16) for _ in range(3)]
            at = ph.sb([64, 4, 64], BF16)
            ot = ph.sb([64, 4, 256], F32)
            sq = ph.sb([64, 4, 256], F32)
            ssq = ph.sb([64, 4], F32)
            pz = ph.ps([64, 512], F32)
            pd = ph.ps([64, 512], F32)
            pbk = ph.ps([128, 512], F32)
            pS = ph.ps([128, 4, 256], F32)
            po = ph.ps([64, 4, 256], F32)
            pa_ps = ph.ps([64, 512], F32)
            nblk = 0
            nch = 0
            pipe = []
            G = dict(tri16=tri16, sp=sp, sp16=sp16, splo=splo, kd=kd, ebl=ebl, qtl=qtl, ktl=ktl, ez=ez, ed=ed, eb=eb, enb=enb,
                     at=at, ot=ot, sq=sq, ssq=ssq, pz=pz, pd=pd, pbk=pbk, pa=pa_ps, pS=pS, po=po, St=St, Sb=Sb, cm=cm,
                     wab=wab, negcol=negcol, o_d=o_d, og_d=og_d)
            for d in range(2):
                for h in range(4):
                    kb.memset("dve", St[h].v(), 0.0)
                    kb.memset("dve", Sb[h].v(), 0.0)
                ctx_blocks = list(range(TOWN // BT, S // BT))
                own_blocks = list(range(0, TOWN // BT))
                if d == 1:
                    ctx_blocks = ctx_blocks[::-1]
                    own_blocks = own_blocks[::-1]
                for (own, blocks) in ((False, ctx_blocks), (True, own_blocks)):
                    for bi in blocks:
                        p0 = bi * BT
                        bb = nblk % 2
                        nblk += 1
                        kB, vB = kblk[bb], vblk[bb]
                        kb.dma(kB.v(), k_d[p0:p0 + BT, :].rr("(c p) n -> p c n", p=64))
                        kb.dma(vB.v(), v_d[p0:p0 + BT, :].rr("(c p) n -> p c n", p=64))
                        aH = a16[bb]
                        kb.dma(aH[0:16, :], a1T_d[16 * d:16 * d + 16, p0:p0 + BT])
                        if own:
                            qB, ktB, oB, rB, ogB = qblk[bb], ktblk[bb], oblk[bb], rblk[bb], ogstg[bb]
                            kb.dma(qB.v(), qT_d[:, p0:p0 + BT].rr("(h p) t -> p h t", p=128))
                            kb.dma(ktB.v(), kT_d[:, p0:p0 + BT].rr("(h p) t -> p h t", p=128))
                            if d == 1:
                                kb.dma(oB.v(), o_d[p0:p0 + BT, :].rr("(c p) n -> p c n", p=64))
                                kb.dma(rB.v(), r_d[p0:p0 + BT, :].rr("(c p) n -> p c n", p=64))
                        js = list(range(NB)) if d == 0 else list(range(NB))[::-1]
                        if not own:
                            qB = ktB = oB = rB = ogB = None
                        for j in js:
                            c3 = nch % 3
                            nch += 1
                            last_ctx = (not own) and bi == blocks[-1] and j == js[-1]
                            blk_end = own and j == js[-1]
                            pipe.append(self._gla_chunk(kb, d, own, j, bi * NB + j, c3, last_ctx, blk_end, p0, BT,
                                                        dict(kB=kB, vB=vB, aH=aH, qB=qB, ktB=ktB, oB=oB, rB=rB, ogB=ogB), G))
                            n_ = len(pipe)
                            pipe[n_ - 1][0]()
                            if n_ >= 2:
                                pipe[n_ - 2][1]()
                            if n_ >= 3:
                                pipe[n_ - 3][2]()
                n_ = len(pipe)
                pipe[n_ - 1][1]()
                pipe[n_ - 2][2]()
                pipe[n_ - 1][2]()
                pipe = []

    def _gla_chunk(self, kb, d, own, j, chunk, c3, last_ctx, blk_end, p0, BT, B, G):
        tcol = lambda kind: self.tri[:, (d * 3 + kind) * 64:(d * 3 + kind) * 64 + 64]
        t16 = lambda kind: G["tri16"][:, (d * 3 + kind) * 64:(d * 3 + kind) * 64 + 64]
        sp_, sph, spl, kd_, ebl_, qt_, kt_ = G["sp"][c3], G["sp16"][c3], G["splo"][c3], G["kd"][c3], G["ebl"][c3], G["qtl"][c3], G["ktl"][c3]
        ez, ed, eb, enb, at, ot, sq, ssq = G["ez"], G["ed"], G["eb"], G["enb"], G["at"], G["ot"], G["sq"], G["ssq"]
        pz, pd, pbk, pa_, pS, po = G["pz"], G["pd"], G["pbk"], G["pa"], G["pS"], G["po"]
        St, Sb, cm = G["St"], G["Sb"], G["cm"]
        kB, vB, aH, qB, ktB, oB, rB, ogB = [B[k] for k in ("kB", "vB", "aH", "qB", "ktB", "oB", "rB", "ogB")]

        def A1():
            kb.mm(pz.v(), aH[:, j * 64:(j + 1) * 64], G["wab"][d].v())
            kb.act(ez.v(), pz.v(), AF.Exp, scale=-1.0)
            kb.act(sp_.v(), ez.v(), AF.Ln, bias=1.0)
            kb.act(sph.v(), ez.v(), AF.Ln, bias=1.0)
            kb.tt("dve", spl.v(), sp_.v(), sph.v(), ALU.subtract)

        def A2():
            kb.mm(pd.v(), t16(1), sph.v(), start=True, stop=False)
            kb.mm(pd.v(), t16(1), spl.v(), start=False, stop=True)
            if own:
                kb.act(ed.v(), pd.v(), AF.Exp)
            else:
                kb.act(ed.v(), pd.v(), AF.Exp, bias=cm[:, d * 256 + chunk:d * 256 + chunk + 1])
            kb.tt("dve", kd_.v(), kB[:, j, :], ed.v(), ALU.mult)
            if own:
                for h in range(4):
                    kb.mm(pbk[:, h * 64:(h + 1) * 64], sph[:, h * 128:(h + 1) * 128], t16(0), start=True, stop=False)
                    kb.mm(pbk[:, h * 64:(h + 1) * 64], spl[:, h * 128:(h + 1) * 128], t16(0), start=False, stop=True)
                pbv = pbk[:, 0:256].rr("p (h t) -> p h t", h=4)
                kb.act(eb.v(), pbv, AF.Exp)
                kb.act(enb.v(), pbv, AF.Exp, scale=-1.0)
                last = 63 if d == 0 else 0
                kb.cp("pool", ebl_.v(), eb[:, :, last])
                kb.stt("dve", qt_.v(), qB[:, :, j * 64:(j + 1) * 64], 128.0 ** -0.5, eb.v(), ALU.mult, ALU.mult)
                kb.tt("pool", kt_.v(), ktB[:, :, j * 64:(j + 1) * 64], enb.v(), ALU.mult)
            else:
                for h in range(4):
                    kb.mm(pbk[:, h:h + 1], sph[:, h * 128:(h + 1) * 128], G["negcol"].v())
                kb.act(ebl_.v(), pbk[:, 0:4], AF.Exp)

        def Bs():
            if own:
                pa = pa_[0:64, 0:256].rr("p (h t) -> p h t", h=4)
                for h in range(4):
                    kb.mm(pa[:, h, :], kt_[:, h, :], qt_[:, h, :])
                kb.tt("dve", at.v(), pa, tcol(2).us(1).bc([64, 4, 64]), ALU.mult)
                for h in range(4):
                    kb.mm(po[:, h, :], at[:, h, :], vB[:, j, h * 256:(h + 1) * 256], start=True, stop=False)
                    kb.mm(po[:, h, :], qt_[:, h, :], Sb[h].v(), start=False, stop=True)
                if d == 0:
                    kb.cp("act", oB[:, j, :], po.v().rr("p h e -> p (h e)"))
                else:
                    kb.tt("dve", ot.v(), po.v(), oB[:, j, :].rr("p (h e) -> p h e", h=4), ALU.add)
                    for h in range(4):
                        kb.act(sq[:, h, :], ot[:, h, :], AF.Square, accum=ssq[:, h:h + 1])
                    kb.ts("dve", ssq.v(), ssq.v(), 1.0 / 256.0, EPS, op0=ALU.mult, op1=ALU.add)
                    kb.act(ssq.v(), ssq.v(), AF.Sqrt)
                    kb.recip(ssq.v(), ssq.v())
                    for h in range(4):
                        kb.stt("dve", ot[:, h, :], ot[:, h, :], ssq[:, h:h + 1], self.gngb.v(), ALU.mult, ALU.mult)
                    kb.tt("dve", ogB[:, j, :], ot.v().rr("p h e -> p (h e)"), rB[:, j, :], ALU.mult)
            for h in range(4):
                kb.mm(pS[:, h, :], kd_[:, h * 128:(h + 1) * 128], vB[:, j, h * 256:(h + 1) * 256])
            for h in range(4):
                kb.stt("dve", St[h].v(), St[h].v(), ebl_[:, h:h + 1], pS[:, h, :], ALU.mult, ALU.add)
                if own or last_ctx:
                    kb.cp("act", Sb[h].v(), St[h].v())
            if blk_end:
                if d == 0:
                    kb.dma(G["o_d"][p0:p0 + BT, :].rr("(c p) n -> p c n", p=64), oB.v(), q="pool")
                else:
                    kb.dma(G["og_d"][p0:p0 + BT, :].rr("(c p) n -> p c n", p=64), ogB.v(), q="pool")

        return (A1, A2, Bs)

    def cvt_load(self, kb, ph, dst, src, n, stg):
        raise NotImplementedError

    def s_mix(self, kb, gp):
        self.s_mix_a(kb, gp)
        self.s_mix_b(kb, gp)
        self.s_mix_c(kb, gp)

    def load_w_bf16(self, kb, ph, dstbuf, src, nk, stg, n0=0):
        engs = ["dve", "act"]
        for kc in range(nk):
            st = stg[(n0 + kc) % len(stg)]
            kb.dma(st.v(), src[kc * 128:(kc + 1) * 128, :])
            kb.cp(engs[kc % 2], dstbuf[:, kc, :], st.v())

    def s_mix_a(self, kb, gp):
        I = self.inp
        UT_d, og_d, m_d = self.scr["UT_d"], self.scr["og_d"], self.scr["m_d"]
        mT_d = self.scratch("mT_d", [D, TOWN], BF16)
        with Phase(kb, "mixa") as ph:
            wgl = ph.sb([128, 8, D], BF16)
            wfp = ph.sb([128, 16, D], BF16)
            with Phase(kb, "mixa0") as ph0:
                stg = [ph0.sb([128, D], F32) for _ in range(3)]
                wfn = ph0.sb([128, 8, D], BF16)
                self.load_w_bf16(kb, ph0, wfn, I["w_fnet"], 8, stg)
                self.load_w_bf16(kb, ph0, wgl, I["w_gla"], 8, stg)
                cct = ph0.sb([128, 2, 512], BF16)
                kb.dma(cct.v(), I["cc"].v().rr("(h p) n -> p h n", p=128))
                pw = [ph0.ps([128, 512], F32) for _ in range(2)]
                nn = 0
                for g in range(4):
                    for c in range(2):
                        for hh in range(2):
                            for cc in range(4):
                                p = pw[nn % 2]
                                nn += 1
                                for half in range(2):
                                    kb.mm(p.v(), cct[:, half, c * 256 + hh * 128:c * 256 + hh * 128 + 128],
                                          wfn[:, g * 2 + half, cc * 512:(cc + 1) * 512], start=(half == 0), stop=(half == 1))
                                kb.cp("act" if nn % 2 == 0 else "dve", wfp[:, (g * 2 + hh) * 2 + c, cc * 512:(cc + 1) * 512], p.v())
            utT = [ph.sb([128, 8, 2, 128], BF16) for _ in range(2)]
            ogt = [ph.sb([128, 1024], BF16) for _ in range(2)]
            sgt = [ph.sb([128, 4096], BF16) for _ in range(2)]
            ogT = ph.sb([128, 8, 128], BF16)
            ta = ph.sb([128, 1024], F32)
            tb = ph.sb([128, 1024], F32)
            mg = ph.sb([128, D], BF16)
            mstg = [ph.sb([128, 16, 512], BF16) for _ in range(2)]
            pyf = ph.ps([128, 1024], F32)
            pyg = ph.ps([128, 1024], F32)
            pog = ph.ps([128, 1024], BF16)
            pmt = ph.ps([128, 2048], BF16)
            for tl in range(32):
                t0 = tl * 128
                u_, o_, s_ = utT[tl % 2], ogt[tl % 2], sgt[tl % 2]
                for c_ in range(2):
                    kb.dma(u_[:, :, c_, :], UT_d[:, :, c_, t0:t0 + 128].rr("b p t -> p b t"))
                kb.dma(o_.v(), og_d[t0:t0 + 128, :])
                kb.dma(s_.v(), m_d[t0:t0 + 128, :])
                for kc in range(8):
                    kb.tr(pog[:, kc * 128:(kc + 1) * 128], o_[:, kc * 128:(kc + 1) * 128], self.identb.v())
                kb.cp("act", ogT.v().rr("p k t -> p (k t)"), pog.v())
                for hf in range(2):
                    for cc in range(2):
                        c0 = hf * 1024 + cc * 512
                        for kk in range(16):
                            kb.mm(pyf[:, cc * 512:(cc + 1) * 512], u_[:, kk // 2, kk % 2, :], wfp[:, kk, c0:c0 + 512],
                                  start=(kk == 0), stop=(kk == 15))
                        for kk in range(8):
                            kb.mm(pyg[:, cc * 512:(cc + 1) * 512], ogT[:, kk, :], wgl[:, kk, c0:c0 + 512],
                                  start=(kk == 0), stop=(kk == 7))
                    kb.tt("dve", ta.v(), pyf.v(), s_[:, hf * 1024:(hf + 1) * 1024], ALU.mult)
                    kb.tt("dve", tb.v(), pyg.v(), s_[:, 2048 + hf * 1024:2048 + (hf + 1) * 1024], ALU.mult)
                    kb.tt("dve", mg[:, hf * 1024:(hf + 1) * 1024], ta.v(), tb.v(), ALU.add)
                for kc in range(16):
                    kb.tr(pmt[:, kc * 128:(kc + 1) * 128], mg[:, kc * 128:(kc + 1) * 128], self.identb.v())
                ms = mstg[(tl // 4) % 2]
                kb.cp("act", ms[:, :, (tl % 4) * 128:(tl % 4) * 128 + 128], pmt.v().rr("p (k t) -> p k t", k=16))
                if tl % 4 == 3:
                    tt0 = (tl // 4) * 512
                    kb.dma(mT_d[:, tt0:tt0 + 512].rr("(k p) t -> p k t", p=128), ms.v(), q="pool")

    def s_mix_b(self, kb, gp):
        I = self.inp
        mT_d = self.scr["mT_d"]
        h_d = self.scratch("h_d", [TOWN, D], F32)
        n2T_d = self.scratch("n2T_d", [D, TOWN], BF16)
        with Phase(kb, "mixb") as ph:
            wo = ph.sb([128, 16, D], BF16)
            with Phase(kb, "mixb0") as ph0:
                stg = [ph0.sb([128, D], F32) for _ in range(2)]
                self.load_w_bf16(kb, ph0, wo, I["w_out"], 16, stg)
            mT = [ph.sb([128, 16, 512], BF16) for _ in range(1)]
            ht = ph.sb([128, 4, D], F32)
            xt = ht
            tmp = ph.sb([128, D], F32)
            xb = ph.sb([128, 4, D], BF16)
            junk = tmp
            ss = ph.sb([128, 4], F32)
            rstd = ph.sb([128, 4], F32)
            nT = ph.sb([128, 16, 512], BF16)
            pmix = ph.ps([128, D], F32)
            pst = [ph.ps([128, 1024], BF16) for _ in range(2)]
            g1b = ph.sb([128, D], F32)
            self.g1b = g1b
            with Phase(kb, "mixb1") as ph1:
                rep = ph1.sb([128, 16, 128], F32)
                kb.cp("dve", rep.v(), self.g1.us(2).bc([128, 16, 128]))
                for kc in range(16):
                    kb.mm(pmix[:, kc * 128:(kc + 1) * 128], rep[:, kc, :], self.identf.v())
                kb.cp("act", g1b.v(), pmix.v())
            for tl in range(8):
                t0 = tl * 512
                m_ = mT[0]
                kb.dma(m_.v(), mT_d[:, t0:t0 + 512].rr("(k p) t -> p k t", p=128))
                kb.dma(xt.v(), I["x_own"][t0:t0 + 512, :].rr("(s p) d -> p s d", p=128))
                for s in range(4):
                    for cc in range(4):
                        for kc in range(16):
                            kb.mm(pmix[:, cc * 512:(cc + 1) * 512], m_[:, kc, s * 128:(s + 1) * 128], wo[:, kc, cc * 512:(cc + 1) * 512],
                                  start=(kc == 0), stop=(kc == 15))
                    kb.tt("dve", tmp.v(), pmix.v(), self.g1b.v(), ALU.mult)
                    kb.tt("dve", ht[:, s, :], tmp.v(), xt[:, s, :], ALU.add)
                kb.dma(h_d[t0:t0 + 512, :].rr("(s p) d -> p s d", p=128), ht.v(), q="pool")
                self.norm_transpose(kb, ph, ht, 4, self.gam2, self.sh2, nT, pst, xb, ss, rstd, junk)
                kb.dma(n2T_d[:, t0:t0 + 512].rr("(k p) t -> p k t", p=128), nT.v(), q="pool")

    def s_mix_c(self, kb, gp):
        I = self.inp
        n2T_d = self.scr["n2T_d"]
        eT_d = self.scratch("eT_d", [3, 128, TOWN], F32)
        with Phase(kb, "mixc") as ph:
            wq = ph.sb([128, 16, D], BF16)
            keysT = ph.sb([128, 16, 128], BF16)
            with Phase(kb, "mixc0") as ph0:
                stg = [ph0.sb([128, D], F32) for _ in range(2)]
                self.load_w_bf16(kb, ph0, wq, I["peer_w_q"], 16, stg)
                kf = ph0.sb([128, D], F32)
                kb.dma(kf.v().rr("p (a d) -> p a d", a=16), I["peer_keys"].v().rr("a k d -> k a d"))
                kbf = ph0.sb([128, 16, 128], BF16)
                kb.cp("dve", kbf.v().rr("p a d -> p (a d)"), kf.v())
                pkt = ph0.ps([128, 2048], BF16)
                for a in range(16):
                    kb.tr(pkt[:, a * 128:(a + 1) * 128], kbf[:, a, :], self.identb.v())
                kb.cp("act", keysT.v().rr("p a k -> p (a k)"), pkt.v())
            n2T = ph.sb([128, 16, 512], BF16)
            qTs = [ph.sb([128, 16, 512], BF16) for _ in range(2)]
            scs = [ph.sb([128, 16, 128], F32) for _ in range(2)]
            tmpm = ph.sb([128, 16, 128], F32)
            v16 = ph.sb([128, 16, 16], F32)
            i16 = ph.sb([128, 16, 16], U32)
            idxf = ph.sb([128, 16, 16], F32)
            cand = ph.sb([128, 8, 256], F32)
            cand2 = Buf(tmpm.ap.rearrange("p a k -> p (a k)").rearrange("p (h c) -> p h c", h=8))
            sv = ph.sb([128, 8, 16], F32)
            pos = ph.sb([128, 8, 16], U32)
            pq = ph.sb([128, 2, 128], U32)
            pqf = ph.sb([128, 2, 128], F32)
            oh = ph.sb([128, 128, 16], F32)
            e12 = ph.sb([128, 2, 128], F32)
            gex = ph.sb([128, 8, 16], F32)
            gz = ph.sb([128, 8], F32)
            gg = ph.sb([128, 128], F32)
            estg = [ph.sb([128, 3, 512], F32) for _ in range(1)]
            v16s = [Buf(v16.ap[:, cc, :]) for cc in range(16)]
            i16s = [Buf(i16.ap[:, cc, :]) for cc in range(16)]
            tmps = [Buf(tmpm.ap[:, cc, :]) for cc in range(16)]
            svs = [Buf(sv.ap[:, h, :]) for h in range(8)]
            poss = [Buf(pos.ap[:, h, :]) for h in range(8)]
            c2s = [Buf(cand2.ap[:, h, :]) for h in range(8)]
            pq_ = [ph.ps([128, 512], F32) for _ in range(2)]
            psc = ph.ps([128, 2048], F32)
            ptr = ph.ps([128, 512], F32)
            dv = kb.eng["dve"]
            iota16 = self.iota[:, 0:16]
            def emit_qT(tl, part):
                t0 = tl * 512
                if part == 0:
                    kb.dma(n2T.v(), n2T_d[:, t0:t0 + 512].rr("(k p) t -> p k t", p=128))
                q_ = qTs[tl % 2]
                for cc in range(part * 4, part * 4 + 4):
                    p = pq_[cc % 2]
                    for kc in range(16):
                        kb.mm(p.v(), wq[:, kc, cc * 128:(cc + 1) * 128], n2T[:, kc, :], start=(kc == 0), stop=(kc == 15))
                    kb.cp("act", q_[:, cc, :], p.v())

            def emit_scores(tl, s):
                q_ = qTs[tl % 2]
                sc = scs[(tl * 4 + s) % 2]
                for cc in range(16):
                    kb.mm(psc[:, cc * 128:(cc + 1) * 128], q_[:, cc, s * 128:(s + 1) * 128], keysT[:, cc, :])
                kb.cp("act", sc.v().rr("p a k -> p (a k)"), psc.v())

            def emit_chain_tail(tl, s):
                t0 = tl * 512
                sc = scs[(tl * 4 + s) % 2]
                es_ = estg[0]
                for cc in range(16):
                    kb.op("dve", [sc.v()], [v16s[cc].v()], lambda g, cc=cc: g.max(out=v16[:, cc, 0:8].ap, in_=sc[:, cc, :].ap))
                for cc in range(16):
                    kb.op("dve", [sc.v(), v16s[cc].v()], [i16s[cc].v()], lambda g, cc=cc: g.max_index(out=i16[:, cc, 0:8].ap, in_max=v16[:, cc, 0:8].ap, in_values=sc[:, cc, :].ap))
                for cc in range(16):
                    kb.op("dve", [sc.v(), v16s[cc].v()], [tmps[cc].v()], lambda g, cc=cc: g.match_replace(out=tmpm[:, cc, :].ap, in_to_replace=v16[:, cc, 0:8].ap, in_values=sc[:, cc, :].ap, imm_value=-1e30))
                for cc in range(16):
                    kb.op("dve", [tmps[cc].v()], [v16s[cc].v()], lambda g, cc=cc: g.max(out=v16[:, cc, 8:16].ap, in_=tmpm[:, cc, :].ap))
                for cc in range(16):
                    kb.op("dve", [tmps[cc].v(), v16s[cc].v()], [i16s[cc].v()], lambda g, cc=cc: g.max_index(out=i16[:, cc, 8:16].ap, in_max=v16[:, cc, 8:16].ap, in_values=tmpm[:, cc, :].ap))
                kb.cp("dve", idxf.v(), V(i16, i16.ap, i16s))
                v4 = V(v16, v16.ap, v16s).rr("p (h f) k -> p h f k", f=2)
                kb.tt("dve", cand.v().rr("p h (a b) -> p h a b", a=16), v4[:, :, 0, :].us(3).bc([128, 8, 16, 16]),
                      v4[:, :, 1, :].us(2).bc([128, 8, 16, 16]), ALU.add)
                for h in range(8):
                    kb.op("dve", [cand.v()], [svs[h].v()], lambda g, h=h: g.max(out=sv[:, h, 0:8].ap, in_=cand[:, h, :].ap))
                for h in range(8):
                    kb.op("dve", [cand.v(), svs[h].v()], [poss[h].v()], lambda g, h=h: g.max_index(out=pos[:, h, 0:8].ap, in_max=sv[:, h, 0:8].ap, in_values=cand[:, h, :].ap))
                for h in range(8):
                    kb.op("dve", [cand.v(), svs[h].v()], [c2s[h].v()], lambda g, h=h: g.match_replace(out=cand2[:, h, :].ap, in_to_replace=sv[:, h, 0:8].ap, in_values=cand[:, h, :].ap, imm_value=-1e30))
                for h in range(8):
                    kb.op("dve", [c2s[h].v()], [svs[h].v()], lambda g, h=h: g.max(out=sv[:, h, 8:16].ap, in_=cand2[:, h, :].ap))
                for h in range(8):
                    kb.op("dve", [c2s[h].v(), svs[h].v()], [poss[h].v()], lambda g, h=h: g.max_index(out=pos[:, h, 8:16].ap, in_max=sv[:, h, 8:16].ap, in_values=cand2[:, h, :].ap))
                posv = pos.v().rr("p h k -> p (h k)")
                posw = V(pos, pos.ap, poss)
                svw = V(sv, sv.ap, svs)
                kb.op("dve", [posw], [pq.v()], lambda g: g.tensor_single_scalar(out=pq[:, 0, :].ap, in_=posv.ap, scalar=4, op=ALU.logical_shift_right))
                kb.op("dve", [posw], [pq.v()], lambda g: g.tensor_single_scalar(out=pq[:, 1, :].ap, in_=posv.ap, scalar=15, op=ALU.bitwise_and))
                kb.cp("dve", pqf.v(), pq.v())
                i4 = idxf.v().rr("p (h f) k -> p h f k", f=2)
                for f_ in range(2):
                    kb.tt("dve", oh.v(), pqf[:, f_, :].us(2).bc([128, 128, 16]), iota16.us(1).bc([128, 128, 16]), ALU.is_equal)
                    kb.tt("dve", oh.v().rr("p (h k) a -> p h k a", h=8), oh.v().rr("p (h k) a -> p h k a", h=8),
                          i4[:, :, f_, :].us(2).bc([128, 8, 16, 16]), ALU.mult)
                    kb.op("dve", [oh.v()], [e12.v()], lambda g, f_=f_: g.tensor_reduce(out=e12[:, f_, :].ap, in_=oh.v().ap, axis=AX.X, op=ALU.add))
                kb.tt("dve", gex.v(), svw, svw[:, :, 0:1].bc([128, 8, 16]), ALU.subtract)
                kb.act(gex.v(), gex.v(), AF.Exp)
                kb.op("dve", [gex.v()], [gz.v()], lambda g: g.tensor_reduce(out=gz.v().ap, in_=gex.v().ap, axis=AX.X, op=ALU.add))
                kb.recip(gz.v(), gz.v())
                kb.tt("dve", gg.v().rr("p (h k) -> p h k", h=8), gex.v(), gz.v().us(2).bc([128, 8, 16]), ALU.mult)
                for j, src in enumerate((e12[:, 0, :], e12[:, 1, :], gg.v())):
                    kb.mm(ptr[:, j * 128:(j + 1) * 128], src, self.identf.v())
                kb.cp("act", es_[:, :, s * 128:(s + 1) * 128], ptr[:, 0:384].rr("p (j t) -> p j t", j=3))
                if s == 3:
                    kb.dma(eT_d[:, :, t0:t0 + 512].rr("j p t -> p j t"), es_.v(), q="pool")

            for part in range(4):
                emit_qT(0, part)
            emit_scores(0, 0)
            for tl in range(8):
                for s in range(4):
                    if tl + 1 < 8:
                        emit_qT(tl + 1, s)
                    if s < 3:
                        emit_scores(tl, s + 1)
                    elif tl + 1 < 8:
                        emit_scores(tl + 1, 0)
                    emit_chain_tail(tl, s)

    def s_peer_prep(self, kb, gp):
        I = self.inp
        uT_d = self.scratch("uT_d", [128, 128, 16, 128], BF16)
        vb_d = self.scratch("vb_d", [16384, D], BF16)
        with Phase(kb, "pprep") as ph:
            ust = [ph.sb([128, D], F32) for _ in range(2)]
            vst = [ph.sb([128, D], F32) for _ in range(2)]
            ub = [ph.sb([128, D], BF16) for _ in range(2)]
            vb = [ph.sb([128, 4, D], BF16) for _ in range(2)]
            uo = [ph.sb([128, 4, 16, 128], BF16) for _ in range(2)]
            pt = [ph.ps([128, 2048], BF16) for _ in range(2)]
            for c in range(128):
                g, ci = c // 4, c % 4
                u_, v_ = ust[c % 2], vst[c % 2]
                kb.dma(u_.v(), I["peer_u"][c * 128:(c + 1) * 128, :])
                kb.dma(v_.v(), I["peer_v"][c * 128:(c + 1) * 128, :])
                kb.cp("dve", ub[c % 2].v(), u_.v())
                kb.cp("act" if c % 2 == 0 else "dve", vb[g % 2][:, ci, :], v_.v())
                p = pt[c % 2]
                for kc in range(16):
                    kb.tr(p[:, kc * 128:(kc + 1) * 128], ub[c % 2][:, kc * 128:(kc + 1) * 128], self.identb.v())
                kb.cp("act", uo[g % 2][:, ci, :, :].rr("p k e -> p (k e)"), p.v())
                if ci == 3:
                    kb.dma(uT_d[g * 4:(g + 1) * 4].rr("c p k e -> p c (k e)"), uo[g % 2].v().rr("p c k e -> p c (k e)"), q="pool")
                    kb.dma(vb_d[g * 512:(g + 1) * 512, :].rr("(c p) n -> p c n", p=128), vb[g % 2].v(), q="pool")

    def s_peer(self, kb, gp):
        I = self.inp
        uT_d, vb_d, n2T_d, eT_d, h_d = [self.scr[k] for k in ("uT_d", "vb_d", "n2T_d", "eT_d", "h_d")]
        TT = 256
        with Phase(kb, "peer") as ph:
            n2T = ph.sb([128, 16, TT], BF16)
            eT = ph.sb([128, 3, TT], F32)
            eTb = ph.sb([128, 2, TT], BF16)
            iob = ph.sb([128, 128], BF16)
            kb.cp("dve", iob.v(), self.iota.v())
            AC = ph.sb([128, 128, TT], BF16)
            Lbs = [ph.sb([128, 16, 128], BF16) for _ in range(2)]
            REs = [ph.sb([128, 2, 16, 128], BF16) for _ in range(2)]
            us = [ph.sb([128, 2, 16, 128], BF16) for _ in range(4)]
            vs = [ph.sb([128, 4, 1024], BF16) for _ in range(3)]
            hout = ph.sb([128, 2, D], F32)
            tmp = ph.sb([128, D], F32)
            ss = ph.sb([128, 2], F32)
            pu = [ph.ps([128, 512], F32) for _ in range(2)]
            pvs = [ph.ps([128, 1024], F32) for _ in range(2)]
            nu = nv = ng = 0
            for tl in range(TOWN // TT):
                t0 = tl * TT
                kb.dma(n2T.v(), n2T_d[:, t0:t0 + TT].rr("(k p) t -> p k t", p=128))
                kb.dma(eT.v(), eT_d[:, :, t0:t0 + TT].rr("j p t -> p j t"))
                kb.dma(hout.v(), h_d[t0:t0 + TT, :].rr("(s p) d -> p s d", p=128))
                def build_lr(st_):
                    Lb_, RE_ = Lbs[st_ % 2], REs[st_ % 2]
                    a0 = st_ * 16
                    io = iob.v().us(1).us(1).bc([128, 2, 16, 128])
                    kb.tt("dve", RE_.v(), eTb[:, :, a0:a0 + 16].us(3).bc([128, 2, 16, 128]), io, ALU.is_equal)
                    kb.tt("pool", Lb_.v(), RE_[:, 0, :, :], eT[:, 2, a0:a0 + 16].us(2).bc([128, 16, 128]), ALU.mult)
                kb.cp("act", eTb.v(), eT[:, 0:2, :])
                build_lr(0)
                for cg in range(64):
                    u_ = us[nu % 4]
                    nu += 1
                    kb.dma(u_.v().rr("p c k e -> p c (k e)"), uT_d[cg * 2:(cg + 1) * 2].rr("c p k e -> p c (k e)"))
                    for ci in range(2):
                        c = cg * 2 + ci
                        p = pu[c % 2]
                        for kc in range(16):
                            kb.mm(p[:, 0:TT], u_[:, ci, kc, :], n2T[:, kc, :], start=(kc == 0), stop=(kc == 15))
                        kb.act(AC[:, c, :], p[:, 0:TT], AF.Gelu)
                for st in range(TT // 16):
                    ts0 = st * 16
                    if st + 1 < TT // 16:
                        build_lr(st + 1)
                    Lb, Rb = Lbs[st % 2], REs[st % 2][:, 1, :, :]
                    for tg in range(2):
                        p = pvs[ng % 2]
                        ng += 1
                        pgv = p.v().rr("p (t c) -> p c t", t=8)
                        for t in range(8):
                            tt_ = tg * 8 + t
                            kb.mm(p[:, t * 128:(t + 1) * 128], Rb[:, tt_, :], Lb[:, tt_, :])
                        tk = ts0 + tg * 8
                        acv = AC[:, :, tk:tk + 8]
                        kb.tt("dve", acv, acv, pgv, ALU.mult)
                for half in range(2):
                    for cg in range(32):
                        v_ = vs[nv % 3]
                        nv += 1
                        kb.dma(v_.v(), vb_d[cg * 512:(cg + 1) * 512, half * 1024:(half + 1) * 1024].rr("(c p) n -> p c n", p=128))
                        for ci in range(4):
                            c = cg * 4 + ci
                            for ts in range(2):
                                for cc in range(2):
                                    kb.mm(pvs[ts][:, cc * 512:(cc + 1) * 512], AC[:, c, ts * 128:(ts + 1) * 128], v_[:, ci, cc * 512:(cc + 1) * 512],
                                          start=(c == 0), stop=(c == 127))
                    for ts in range(2):
                        hc = slice(half * 1024, (half + 1) * 1024)
                        kb.tt("dve", tmp[:, 0:1024], pvs[ts].v(), self.g2b[:, hc], ALU.mult)
                        kb.tt("dve", hout[:, ts, hc], tmp[:, 0:1024], hout[:, ts, hc], ALU.add)
                for ts in range(2):
                    kb.act(tmp.v(), hout[:, ts, :], AF.Square, accum=ss[:, ts:ts + 1])
                kb.ts("dve", ss.v(), ss.v(), 1.0 / D, EPS, op0=ALU.mult, op1=ALU.add)
                kb.act(ss.v(), ss.v(), AF.Sqrt)
                kb.recip(ss.v(), ss.v())
                for ts in range(2):
                    kb.stt("dve", hout[:, ts, :], hout[:, ts, :], ss[:, ts:ts + 1], self.fngb.v(), ALU.mult, ALU.mult)
                kb.dma(self.out[t0:t0 + TT, :].rr("(s p) d -> p s d", p=128), hout.v(), q="pool")


def make_in_maps(inputs):
    x = np.asarray(inputs["x"], dtype=np.float32)
    sq = lambda a: np.ascontiguousarray(np.asarray(a, dtype=np.float32))
    shared = {
        "w_ada": sq(inputs["w_ada"][0]), "b_ada": sq(inputs["b_ada"]).reshape(1, -1),
        "norm1_g": sq(inputs["norm1_g"]).reshape(1, -1), "w_in": sq(inputs["w_in"][0]),
        "w_fnet": sq(inputs["w_fnet"][0]), "gla_w_a2": sq(inputs["gla_w_a2"][0]), "gla_b_a": sq(inputs["gla_b_a"][0]),
        "gla_norm_g": sq(inputs["gla_norm_g"]).reshape(1, -1), "w_gla": sq(inputs["w_gla"][0]),
        "w_out": sq(inputs["w_out"][0]), "norm2_g": sq(inputs["norm2_g"]).reshape(1, -1),
        "peer_w_q": sq(inputs["peer_w_q"][0]), "peer_keys": sq(inputs["peer_keys"][0]).reshape(16, 128, 128),
        "peer_u": sq(inputs["peer_u"][0]), "peer_v": sq(inputs["peer_v"][0]),
        "final_norm_g": sq(inputs["final_norm_g"]).reshape(1, -1),
    }
    consts = [host_consts(r) for r in range(4)]
    maps = []
    for c in range(NCORES):
        b, r = c // 4, c % 4
        T0 = TOWN * r
        m = dict(shared)
        m["xs"] = np.ascontiguousarray(np.roll(x[b], -T0, axis=0))
        m["x_own"] = np.ascontiguousarray(x[b, T0:T0 + TOWN])
        m["c"] = sq(inputs["c"][b:b + 1])
        m.update(consts[r])
        maps.append(m)
    return maps


def run_prog(inputs, stop_after=None, debug=(), cores=NCORES):
    p = Prog(stop_after=stop_after, debug=debug)
    nc = p.build()
    maps = make_in_maps(inputs)[:cores]
    res = run_bass_kernel_spmd(nc, maps, core_ids=list(range(cores)))
    return p, res


def kernel(**inputs):
    p, res = run_prog(inputs)
    x = np.asarray(inputs["x"])
    out = np.empty(x.shape, dtype=np.float32)
    for c in range(NCORES):
        b, r = c // 4, c % 4
        out[b, TOWN * r:TOWN * (r + 1)] = res.results[c]["out"]
    return out
```

```python
import math
from contextlib import ExitStack
import numpy as np
import ml_dtypes
import concourse.bass as bass
import concourse.mybir as mybir
from concourse.bass_utils import run_bass_kernel_spmd

F32 = mybir.dt.float32
BF16 = mybir.dt.bfloat16
U32 = mybir.dt.uint32
I32 = mybir.dt.int32
AF = mybir.ActivationFunctionType
ALU = mybir.AluOpType
AX = mybir.AxisListType

D = 2048
S = 16384
TOWN = 4096
NCORES = 8
EPS = 1e-6
SEM_CAP = 30000


class Buf:
    def __init__(self, ap):
        self.ap = ap
        self.w = {}
        self.r = {}
        self.dsem = None
        self.dcnt = 0

    def __getitem__(self, k):
        return V(self, self.ap[k])

    def v(self):
        return V(self, self.ap)


class V:
    def __init__(self, buf, ap, extra=()):
        self.buf = buf
        self.ap = ap
        self.extra = tuple(extra)

    def rr(self, s, **kw):
        return V(self.buf, self.ap.rearrange(s, **kw), self.extra)

    def bc(self, shape):
        return V(self.buf, self.ap.broadcast_to(shape), self.extra)

    def us(self, ax):
        return V(self.buf, self.ap.unsqueeze(ax), self.extra)

    def bitcast(self, dt):
        return V(self.buf, self.ap.bitcast(dt), self.extra)

    def __getitem__(self, k):
        return V(self.buf, self.ap[k], self.extra)

    def bufs(self):
        return (self.buf,) + self.extra


class KB:
    def __init__(self, nc, es):
        self.nc = nc
        self.es = es
        self.eng = {"pe": nc.tensor, "act": nc.scalar, "dve": nc.vector, "pool": nc.gpsimd, "sp": nc.sync}
        self.esem = {}
        self.ecnt = {}
        self.waited = {e: {} for e in self.eng}
        self.nsem = 0
        self.allsems = {}
        for e in ("pe", "act", "dve", "pool"):
            self._newsem(e)
        self.ninst = 0

    def sem(self):
        self.nsem += 1
        s = self.es.enter_context(self.nc.semaphore("s%d" % self.nsem))
        self.allsems[s] = 0
        return s

    def _newsem(self, e):
        self.esem[e] = self.sem()
        self.ecnt[e] = 0

    def _wait(self, e, evs):
        w = self.waited[e]
        for sem, val in evs.items():
            if w.get(sem, 0) >= val:
                continue
            self.eng[e].wait_ge(sem, val)
            w[sem] = val

    def _collect(self, e, reads, writes):
        evs = {}
        own = self.esem.get(e)
        for v in reads:
            for b in v.bufs():
                for sem, val in b.w.items():
                    if e == "pe" and sem is own:
                        continue
                    evs[sem] = max(evs.get(sem, 0), val)
        for v in writes:
            for b in v.bufs():
                for sem, val in list(b.w.items()) + list(b.r.items()):
                    if sem is own:
                        continue
                    evs[sem] = max(evs.get(sem, 0), val)
        return evs

    def op(self, e, reads, writes, fn):
        reads = [v for v in reads if isinstance(v, V)]
        self._wait(e, self._collect(e, reads, writes))
        if self.ecnt[e] >= SEM_CAP:
            self._newsem(e)
        inst = fn(self.eng[e])
        self.ecnt[e] += 1
        sem = self.esem[e]
        inst.then_inc(sem, 1)
        self.allsems[sem] = self.ecnt[e]
        for v in reads:
            for b in v.bufs():
                b.r[sem] = self.ecnt[e]
        for v in writes:
            for b in v.bufs():
                b.w[sem] = self.ecnt[e]
        self.ninst += 1

    def dma(self, out, in_, q="sp", **kw):
        self._wait(q, self._collect(q, [in_], [out]))
        b = out.buf
        if b.dsem is None or b.dcnt >= SEM_CAP:
            b.dsem = self.sem()
            b.dcnt = 0
        inst = self.eng[q].dma_start(out=out.ap, in_=in_.ap, **kw)
        b.dcnt += 16
        inst.then_inc(b.dsem, 16)
        self.allsems[b.dsem] = b.dcnt
        for bb in out.bufs():
            bb.w[b.dsem] = b.dcnt
        for bb in in_.bufs():
            bb.r[b.dsem] = b.dcnt
        self.ninst += 1

    def barrier(self):
        evs = dict((s, v) for s, v in self.allsems.items() if v > 0)
        for e in self.eng:
            own = self.esem.get(e)
            self._wait(e, dict((s, v) for s, v in evs.items() if s is not own))

    def mm(self, out, lhsT, rhs, start=True, stop=True):
        self.op("pe", [lhsT, rhs], [out], lambda g: g.matmul(out.ap, lhsT=lhsT.ap, rhs=rhs.ap, start=start, stop=stop))

    def tr(self, out, in_, ident):
        self.op("pe", [in_, ident], [out], lambda g: g.transpose(out.ap, in_.ap, ident.ap))

    def act(self, out, in_, func, bias=None, scale=None, accum=None, e="act"):
        kw = {}
        rd = [in_]
        if bias is not None:
            kw["bias"] = bias.ap if isinstance(bias, V) else bias
            rd.append(bias)
        if scale is not None:
            kw["scale"] = scale.ap if isinstance(scale, V) else scale
            rd.append(scale)
        wr = [out]
        if accum is not None:
            kw["accum_out"] = accum.ap
            wr.append(accum)
        self.op("act", rd, wr, lambda g: g.activation(out=out.ap, in_=in_.ap, func=func, **kw))

    def tt(self, e, out, in0, in1, op):
        self.op(e, [in0, in1], [out], lambda g: g.tensor_tensor(out=out.ap, in0=in0.ap, in1=in1.ap, op=op))

    def ts(self, e, out, in0, s1, s2=None, op0=ALU.mult, op1=None, accum=None):
        rd = [in0, s1, s2]
        a1 = s1.ap if isinstance(s1, V) else s1
        a2 = s2.ap if isinstance(s2, V) else s2
        kw = {}
        if op1 is not None:
            kw["op1"] = op1
        wr = [out]
        if accum is not None:
            kw["accum_out"] = accum.ap
            wr.append(accum)
        self.op(e, rd, wr, lambda g: g.tensor_scalar(out=out.ap, in0=in0.ap, scalar1=a1, scalar2=a2, op0=op0, **kw))

    def stt(self, e, out, in0, scalar, in1, op0, op1):
        sc = scalar.ap if isinstance(scalar, V) else scalar
        self.op(e, [in0, scalar, in1], [out], lambda g: g.scalar_tensor_tensor(out=out.ap, in0=in0.ap, scalar=sc, in1=in1.ap, op0=op0, op1=op1))

    def cp(self, e, out, in_):
        if e == "act":
            self.op(e, [in_], [out], lambda g: g.copy(out=out.ap, in_=in_.ap))
        else:
            self.op(e, [in_], [out], lambda g: g.tensor_copy(out=out.ap, in_=in_.ap))

    def recip(self, out, in_):
        self.op("dve", [in_], [out], lambda g: g.reciprocal(out=out.ap, in_=in_.ap))

    def memset(self, e, out, val):
        self.op(e, [], [out], lambda g: g.memset(out.ap, val))


class Phase:
    def __init__(self, kb, name):
        self.kb = kb
        self.name = name
        self.es = ExitStack()
        self.n = 0

    def __enter__(self):
        self.es.__enter__()
        return self

    def __exit__(self, *a):
        self.kb.barrier()
        return self.es.__exit__(*a)

    def sb(self, shape, dt, name=None):
        self.n += 1
        t = self.es.enter_context(self.kb.nc.sbuf_tensor("%s_%s%d" % (self.name, name or "t", self.n), list(shape), dt))
        return Buf(t[:])

    def ps(self, shape, dt, name=None):
        self.n += 1
        t = self.es.enter_context(self.kb.nc.psum_tensor("%s_%s%d" % (self.name, name or "p", self.n), list(shape), dt))
        return Buf(t[:])


def host_consts(r):
    c = {}
    c["identf"] = np.eye(128, dtype=np.float32)
    c["identb"] = np.eye(128).astype(ml_dtypes.bfloat16)
    t = np.arange(64)
    fw = (t[:, None] <= t[None, :]).astype(np.float32)
    bw = (t[:, None] >= t[None, :]).astype(np.float32)
    tri = np.zeros((64, 2, 3, 64), np.float32)
    for d, m in enumerate((fw, bw)):
        tri[:, d, 0, :] = -m / 16.0
        tri[:, d, 1, :] = -(1.0 - m) / 16.0
        tri[:, d, 2, :] = m
    c["tri"] = tri.reshape(64, 384)
    ih = np.arange(128)
    ang = 2 * np.pi * np.outer(ih, ih) / 128.0
    c["t1"] = (np.concatenate([np.cos(ang), -np.sin(ang)], axis=1) / 128.0).astype(ml_dtypes.bfloat16)
    T0 = TOWN * r
    il = (T0 + np.arange(128)).astype(np.int64)
    sp = (T0 + 128 * np.arange(32)[None, :] + np.arange(128)[:, None]).astype(np.int64)
    ph = (il[:, None, None] * sp[None, :, :]) % S
    a2 = 2 * np.pi * ph / S
    ec, es_ = np.cos(a2), np.sin(a2)
    c["e2a"] = np.concatenate([ec, -es_], axis=2).reshape(128, 128 * 64).astype(ml_dtypes.bfloat16)
    c["e2b"] = np.concatenate([es_, ec], axis=2).reshape(128, 128 * 64).astype(ml_dtypes.bfloat16)
    ch = np.arange(256)
    a3 = 2 * np.pi * np.outer(ch, ch) / 256.0
    c["cc"] = (np.concatenate([np.cos(a3), np.sin(a3)], axis=1) / 16.0).astype(ml_dtypes.bfloat16)
    pos = np.arange(256) * 64
    mf = (pos >= S - TOWN * r).astype(np.float32)
    mb = (pos < TOWN + (3 - r) * TOWN).astype(np.float32)
    c["cmask"] = np.tile(((np.concatenate([mf, mb]) - 1.0) * 30000.0)[None, :], (64, 1)).astype(np.float32)
    c["iota"] = np.tile(np.arange(128, dtype=np.float32)[None, :], (128, 1))
    return c


CONST_SPECS = [("identf", [128, 128], F32), ("identb", [128, 128], BF16), ("tri", [64, 384], F32),
               ("t1", [128, 256], BF16), ("e2a", [128, 8192], BF16), ("e2b", [128, 8192], BF16),
               ("cc", [256, 512], BF16), ("cmask", [64, 512], F32), ("iota", [128, 128], F32)]

IN_SPECS = [("xs", [S, D]), ("c", [1, D]), ("w_ada", [D, 6 * D]), ("b_ada", [1, 6 * D]), ("norm1_g", [1, D]),
            ("w_in", [D, 8224]), ("w_fnet", [1024, D]), ("gla_w_a2", [2, 16, 512]), ("gla_b_a", [2, 512]),
            ("gla_norm_g", [1, 256]), ("w_gla", [1024, D]), ("w_out", [D, D]), ("norm2_g", [1, D]),
            ("peer_w_q", [D, D]), ("peer_keys", [16, 128, 128]), ("peer_u", [16384, D]), ("peer_v", [16384, D]),
            ("final_norm_g", [1, D])]


class Prog:
    def __init__(self, stop_after=None, debug=()):
        self.stop_after = stop_after
        self.debug = set(debug)
        self.nc = nc = bass.Bass("TRN2", target_bir_lowering=False)
        self.inp = {}
        for name, shape in IN_SPECS:
            self.inp[name] = Buf(nc.dram_tensor(name, list(shape), F32, kind="ExternalInput").ap())
        for name, shape, dt in CONST_SPECS:
            self.inp[name] = Buf(nc.dram_tensor(name, list(shape), dt, kind="ExternalInput").ap())
        self.inp["x_own"] = Buf(nc.dram_tensor("x_own", [TOWN, D], F32, kind="ExternalInput").ap())
        self.out = Buf(nc.dram_tensor("out", [TOWN, D], F32, kind="ExternalOutput").ap())
        self.scr = {}

    def scratch(self, name, shape, dt):
        kind = "ExternalOutput" if name in self.debug else "Internal"
        b = Buf(self.nc.dram_tensor(name, list(shape), dt, kind=kind).ap())
        self.scr[name] = b
        return b

    def build(self):
        with ExitStack() as es:
            self.kb = kb = KB(self.nc, es)
            with Phase(kb, "glob") as gp:
                self.gp = gp
                self._build(kb, gp)
                kb.barrier()
        return self.nc

    def _build(self, kb, gp):
        I = self.inp
        stages = [self.s_setup, self.s_wconv, self.s_projA, self.s_dft, self.s_gla, self.s_mix, self.s_peer_prep,
                  self.s_peer]
        for st in stages:
            st(kb, gp)
            if self.stop_after == st.__name__:
                with Phase(kb, "fin") as ph:
                    z = ph.sb([128, D], F32)
                    kb.memset("dve", z.v(), 0.0)
                    for i in range(TOWN // 128):
                        kb.dma(self.out[i * 128:(i + 1) * 128, :], z.v())
                return

    def s_setup(self, kb, gp):
        I = self.inp
        self.identf = gp.sb([128, 128], F32, "identf")
        self.identb = gp.sb([128, 128], BF16, "identb")
        self.tri = gp.sb([64, 384], F32, "tri")
        self.iota = gp.sb([128, 128], F32, "iota")
        kb.dma(self.identf.v(), I["identf"].v())
        kb.dma(self.identb.v(), I["identb"].v())
        kb.dma(self.tri.v(), I["tri"].v())
        kb.dma(self.iota.v(), I["iota"].v())
        self.modT = gp.sb([128, 96], F32, "modT")
        self.gam1 = gp.sb([128, 16], F32, "gam1")
        self.gam2 = gp.sb([128, 16], F32, "gam2")
        self.g2b = gp.sb([128, D], F32, "g2b")
        self.fngb = gp.sb([128, D], F32, "fngb")
        self.gngb = gp.sb([64, 256], F32, "gngb")
        kb.dma(self.fngb.v(), I["final_norm_g"].v().bc([128, D]))
        kb.dma(self.gngb.v(), I["gla_norm_g"].v().bc([64, 256]))
        with Phase(kb, "ada") as ph:
            rows = ph.sb([128, 128], F32)
            rowsb = ph.sb([96, 128], F32)
            kb.dma(rows[0:16, :], I["c"].v().rr("o (k p) -> (o k) p", p=128))
            kb.dma(rows[16:32, :], I["norm1_g"].v().rr("o (k p) -> (o k) p", p=128))
            kb.dma(rows[32:48, :], I["norm2_g"].v().rr("o (k p) -> (o k) p", p=128))
            kb.dma(rowsb.v(), I["b_ada"].v().rr("o (k p) -> (o k) p", p=128))
            pst = ph.ps([128, 512], F32)
            kb.mm(pst[:, 0:48], rows[0:48, :], self.identf[0:48, 0:48])
            kb.mm(pst[:, 48:144], rowsb[0:96, :], self.identf[0:96, 0:96])
            colsT = ph.sb([128, 144], F32)
            kb.cp("dve", colsT.v(), pst[:, 0:144])
            silc = ph.sb([128, 16], F32)
            kb.act(silc.v(), colsT[:, 0:16], AF.Silu)
            psm = ph.ps([128, 96], F32)
            wa = [ph.sb([128, 16, 512], F32) for _ in range(2)]
            self.Wb = self.scratch("Wb", [128, 16, 8224], BF16)
            wst = [ph.sb([128, 16, 512], F32) for _ in range(2)]
            wcb = [ph.sb([128, 16, 512], BF16) for _ in range(2)]

            def wconv_step(i):
                c0 = i * 512
                n = min(512, 8224 - c0)
                s_, c_ = wst[i % 2], wcb[i % 2]
                kb.dma(s_[:, :, 0:n], I["w_in"][:, c0:c0 + n].rr("(k p) c -> p k c", p=128))
                kb.cp("dve", c_[:, 0:8, 0:n], s_[:, 0:8, 0:n])
                kb.cp("act", c_[:, 8:16, 0:n], s_[:, 8:16, 0:n])
                kb.dma(self.Wb[:, :, c0:c0 + n], c_[:, :, 0:n], q="pool")
            for blk in range(24):
                w = wa[blk % 2]
                kb.dma(w.v(), I["w_ada"][:, blk * 512:(blk + 1) * 512].rr("(k p) c -> p k c", p=128))
                if blk < 17:
                    wconv_step(blk)
                for j in range(4):
                    for kc in range(16):
                        kb.mm(psm[:, blk * 4 + j:blk * 4 + j + 1], w[:, kc, j * 128:(j + 1) * 128], silc[:, kc:kc + 1],
                              start=(kc == 0), stop=(kc == 15))
            kb.tt("dve", self.modT.v(), psm.v(), colsT[:, 48:144], ALU.add)
            m = self.modT
            self.sh1, self.sc1, self.g1, self.sh2, self.sc2, self.g2 = [m[:, 16 * i:16 * (i + 1)] for i in range(6)]
            kb.stt("dve", self.gam1.v(), self.sc1, 1.0, colsT[:, 16:32], ALU.add, ALU.mult)
            kb.stt("dve", self.gam2.v(), self.sc2, 1.0, colsT[:, 32:48], ALU.add, ALU.mult)
            rep = ph.sb([128, 16, 128], F32)
            pb = ph.ps([128, D], F32)
            for gsrc, gdst in ((self.g2, self.g2b),):
                kb.cp("dve", rep.v(), gsrc.us(2).bc([128, 16, 128]))
                for kc in range(16):
                    kb.mm(pb[:, kc * 128:(kc + 1) * 128], rep[:, kc, :], self.identf.v())
                kb.cp("act", gdst.v(), pb.v())
        if "dbg_mod" in self.debug:
            d = self.scratch("dbg_mod", [128, 96], F32)
            kb.dma(d.v(), self.modT.v())
            d2 = self.scratch("dbg_g1b", [128, D], F32)
            kb.dma(d2.v(), self.g2b.v())

    def s_wconv(self, kb, gp):
        pass

    def norm_transpose(self, kb, ph, xt, nsub, gam, sh, nT, pst, xb, ss, rstd, junk):
        for s in range(nsub):
            kb.act(junk.v() if junk is not None else xb[:, s, :], xt[:, s, :], AF.Square, accum=ss[:, s:s + 1])
        kb.ts("dve", rstd[:, 0:nsub], ss[:, 0:nsub], 1.0 / D, EPS, op0=ALU.mult, op1=ALU.add)
        kb.act(rstd[:, 0:nsub], rstd[:, 0:nsub], AF.Sqrt)
        kb.recip(rstd[:, 0:nsub], rstd[:, 0:nsub])
        for s in range(nsub):
            if s % 2 == 0:
                kb.ts("dve", xb[:, s, :], xt[:, s, :], rstd[:, s:s + 1], None, op0=ALU.mult)
            else:
                kb.act(xb[:, s, :], xt[:, s, :], AF.Identity, scale=rstd[:, s:s + 1])
        for kc in range(16):
            p = pst[kc % 2]
            for s in range(nsub):
                kb.tr(p[:, s * 128:(s + 1) * 128], xb[:, s, kc * 128:(kc + 1) * 128], self.identb.v())
            if kc % 2 == 0:
                kb.act(nT[:, kc, :], p[:, 0:nsub * 128], AF.Identity, bias=sh[:, kc:kc + 1], scale=gam[:, kc:kc + 1])
            else:
                kb.ts("dve", nT[:, kc, :], p[:, 0:nsub * 128], gam[:, kc:kc + 1], sh[:, kc:kc + 1], op0=ALU.mult, op1=ALU.add)

    def s_projA(self, kb, gp):
        I = self.inp
        zf_d = self.scratch("zf_d", [S, 1024], BF16)
        k_d = self.scratch("k_d", [S, 512], BF16)
        v_d = self.scratch("v_d", [S, 1024], BF16)
        a1T_d = self.scratch("a1T_d", [32, S], BF16)
        qT_d = self.scratch("qT_d", [512, TOWN], BF16)
        kT_d = self.scratch("kT_d", [512, TOWN], BF16)
        r_d = self.scratch("r_d", [TOWN, 1024], BF16)
        m_d = self.scratch("m_d", [TOWN, 4096], BF16)
        ctx_tiles = [("zf", 0, 512, zf_d, 0), ("zf", 512, 512, zf_d, 512), ("k", 1536, 512, k_d, 0),
                     ("v", 2048, 512, v_d, 0), ("v", 2560, 512, v_d, 512), ("a", 4096, 32, a1T_d, 0)]
        own_tiles = ctx_tiles + [("q", 1024, 512, qT_d, 0), ("r", 3072, 512, r_d, 0), ("r", 3584, 512, r_d, 512)] + \
            [("m", 4128 + 512 * i, 512, m_d, 512 * i) for i in range(8)]
        with Phase(kb, "projA") as ph:
            xt = [ph.sb([128, 4, D], F32) for _ in range(2)]
            xbs = [ph.sb([128, 4, D], BF16) for _ in range(2)]
            junk = None
            sss = [ph.sb([128, 4], F32) for _ in range(2)]
            rstds = [ph.sb([128, 4], F32) for _ in range(2)]
            nTs = [ph.sb([128, 16, 512], BF16) for _ in range(2)]
            wt = [ph.sb([128, 16, 512], BF16) for _ in range(3)]
            stg = [ph.sb([128, 4, 512], BF16) for _ in range(2)]
            stgf = ph.sb([32, 512], BF16)
            pst = [ph.ps([128, 1024], BF16) for _ in range(2)]
            pso = [ph.ps([128, 512], F32) for _ in range(4)]
            nw = 0
            no = 0
            ns = 0
            for tile in range(32):
                x_ = xt[tile % 2]
                kb.dma(x_.v(), I["xs"][tile * 512:(tile + 1) * 512, :].rr("(s p) d -> p s d", p=128))
                nT, xb, ss, rstd = nTs[tile % 2], xbs[tile % 2], sss[tile % 2], rstds[tile % 2]
                self.norm_transpose(kb, ph, x_, 4, self.gam1, self.sh1, nT, pst, xb, ss, rstd, junk)
                own = tile < 8
                t0 = tile * 512
                for (kind, c0, n, dst, dc0) in (own_tiles if own else ctx_tiles):
                    w = wt[nw % 3]
                    nw += 1
                    kb.dma(w[:, :, 0:n], self.Wb[:, :, c0:c0 + n])
                    if kind in ("zf", "k", "v", "r", "m"):
                        sg = stg[ns % 2]
                        ns += 1
                        for s in range(4):
                            p = pso[no % 4]
                            no += 1
                            for kc in range(16):
                                kb.mm(p.v(), nT[:, kc, s * 128:(s + 1) * 128], w[:, kc, :], start=(kc == 0), stop=(kc == 15))
                            if kind == "r":
                                kb.act(sg[:, s, :], p.v(), AF.Silu)
                            elif kind == "m":
                                kb.act(sg[:, s, :], p.v(), AF.Sigmoid)
                            else:
                                kb.cp("act" if s % 2 == 0 else "dve", sg[:, s, :], p.v())
                        kb.dma(dst[t0:t0 + 512, dc0:dc0 + 512].rr("(s p) c -> p s c", p=128), sg.v(), q="pool")
                    if kind == "a":
                        p = pso[no % 4]
                        no += 1
                        for kc in range(16):
                            kb.mm(p[0:32, :], w[:, kc, 0:32], nT[:, kc, :], start=(kc == 0), stop=(kc == 15))
                        kb.cp("dve", stgf.v(), p[0:32, :])
                        kb.dma(a1T_d[:, t0:t0 + 512], stgf.v(), q="pool")
                    if kind == "q" or (kind == "k" and own):
                        sg = stg[ns % 2]
                        ns += 1
                        for cc in range(4):
                            p = pso[no % 4]
                            no += 1
                            for kc in range(16):
                                kb.mm(p.v(), w[:, kc, cc * 128:(cc + 1) * 128], nT[:, kc, :], start=(kc == 0), stop=(kc == 15))
                            kb.cp("act" if cc % 2 == 0 else "dve", sg[:, cc, :], p.v())
                        dd = qT_d if kind == "q" else kT_d
                        kb.dma(dd[:, t0:t0 + 512].rr("(c p) t -> p c t", p=128), sg.v(), q="pool")

    def s_dft(self, kb, gp):
        I = self.inp
        zf_d = self.scr["zf_d"]
        UT_d = self.scratch("UT_d", [8, 128, 2, TOWN], BF16)
        zv = zf_d.v().rr("(p r) c -> p r c", p=128)
        with Phase(kb, "dft") as ph:
            t1 = ph.sb([128, 256], BF16)
            e2a = ph.sb([128, 128, 64], BF16)
            e2b = ph.sb([128, 128, 64], BF16)
            kb.dma(t1.v(), I["t1"].v())
            kb.dma(e2a.v(), I["e2a"].v().rr("p (k n) -> p k n", n=64))
            kb.dma(e2b.v(), I["e2b"].v().rr("p (k n) -> p k n", n=64))
            zb = [ph.sb([128, 128, 128], BF16) for _ in range(2)]
            A = ph.sb([128, 128, 2, 128], BF16)
            ut = [ph.sb([128, 2, 32, 128], BF16) for _ in range(1)]
            ps1 = [ph.ps([128, 512], F32) for _ in range(3)]
            ps2 = [ph.ps([128, 512], F32) for _ in range(3)]
            n1 = n2 = 0
            for blk in range(8):
                z = zb[blk % 2]
                for qd in range(4):
                    kb.dma(z[:, qd * 32:(qd + 1) * 32, :], zv[:, qd * 32:(qd + 1) * 32, blk * 128:(blk + 1) * 128])
                for cp_ in range(64):
                    p = ps1[n1 % 3]
                    n1 += 1
                    for j in range(2):
                        ch = cp_ * 2 + j
                        kb.mm(p[:, j * 256:(j + 1) * 256], z[:, :, ch], t1.v())
                    kb.cp("act" if cp_ % 2 == 0 else "dve", A[:, cp_ * 2:cp_ * 2 + 2, :, :].rr("p a b c -> p (a b c)"), p.v())
                u = ut[0]
                for kg in range(16):
                    p = ps2[n2 % 3]
                    n2 += 1
                    for j in range(8):
                        kl = kg * 8 + j
                        kb.mm(p[:, j * 64:(j + 1) * 64], A[:, :, 0, kl], e2a[:, kl, :], start=True, stop=False)
                        kb.mm(p[:, j * 64:(j + 1) * 64], A[:, :, 1, kl], e2b[:, kl, :], start=False, stop=True)
                    kb.cp("act" if kg % 2 == 0 else "dve", u[:, :, :, kg * 8:(kg + 1) * 8],
                          p.v().rr("p (k c h) -> p c h k", k=8, c=2, h=32))
                kb.dma(UT_d[blk].rr("p c t -> p (c t)"), u.v().rr("p c h k -> p (c h k)"), q="pool")

    def s_gla(self, kb, gp):
        I = self.inp
        S_ = self.scr
        k_d, v_d, a1T_d, qT_d, kT_d, r_d = S_["k_d"], S_["v_d"], S_["a1T_d"], S_["qT_d"], S_["kT_d"], S_["r_d"]
        o_d = self.scratch("o_d", [TOWN, 1024], F32)
        og_d = self.scratch("og_d", [TOWN, 1024], BF16)
        NB = 4
        BT = NB * 64
        with Phase(kb, "gla") as ph:
            wabf = [ph.sb([17, 512], F32) for _ in range(2)]
            wab = [ph.sb([17, 512], BF16) for _ in range(2)]
            for d in range(2):
                kb.dma(wabf[d][0:16, :], I["gla_w_a2"][d])
                kb.dma(wabf[d][16:17, :], I["gla_b_a"][d:d + 1, :])
                kb.cp("dve", wab[d].v(), wabf[d].v())
            tri16 = ph.sb([64, 384], BF16)
            kb.cp("dve", tri16.v(), self.tri.v())
            cm = ph.sb([64, 512], F32)
            kb.dma(cm.v(), I["cmask"].v())
            negcol = ph.sb([64, 1], BF16)
            kb.memset("dve", negcol.v(), -1.0 / 16.0)
            St = [ph.sb([128, 256], F32) for _ in range(4)]
            Sb = [ph.sb([128, 256], BF16) for _ in range(4)]
            kblk = [ph.sb([64, NB, 512], BF16) for _ in range(2)]
            vblk = [ph.sb([64, NB, 1024], BF16) for _ in range(2)]
            a16 = [ph.sb([17, BT], BF16) for _ in range(2)]
            for b_ in a16:
                kb.memset("dve", b_.v(), 1.0)
            sp16 = [ph.sb([64, 512], BF16) for _ in range(3)]
            splo = [ph.sb([64, 512], BF16) for _ in range(3)]
            qblk = [ph.sb([128, 4, BT], BF16) for _ in range(2)]
            ktblk = [ph.sb([128, 4, BT], BF16) for _ in range(2)]
            oblk = [ph.sb([64, NB, 1024], F32) for _ in range(2)]
            rblk = [ph.sb([64, NB, 1024], BF16) for _ in range(2)]
            ogstg = [ph.sb([64, NB, 1024], BF16) for _ in range(2)]
            ez = ph.sb([64, 512], F32)
            sp = [ph.sb([64, 512], F32) for _ in range(3)]
            ed = ph.sb([64, 512], F32)
            kd = [ph.sb([64, 512], BF16) for _ in range(3)]
            ebl = [ph.sb([128, 4], F32) for _ in range(3)]
            eb = ph.sb([128, 4, 64], F32)
            enb = ph.sb([128, 4, 64], F32)
            qtl = [ph.sb([128, 4, 64], BF16) for _ in range(3)]
            ktl = [ph.sb([128, 4, 64], BF16) for _ in range(3)]
            at = ph.sb([64, 4, 64], BF16)
            ot = ph.sb([64, 4, 256], F32)
            sq = ph.sb([64, 4, 256], F32)
            ssq = ph.sb([64, 4], F32)
            pz = ph.ps([64, 512], F32)
            pd = ph.ps([64, 512], F32)
            pbk = ph.ps([128, 512], F32)
            pS = ph.ps([128, 4, 256], F32)
            po = ph.ps([64, 4, 256], F32)
            pa_ps = ph.ps([64, 512], F32)
            nblk = 0
            nch = 0
            pipe = []
            G = dict(tri16=tri16, sp=sp, sp16=sp16, splo=splo, kd=kd, ebl=ebl, qtl=qtl, ktl=ktl, ez=ez, ed=ed, eb=eb, enb=enb,
                     at=at, ot=ot, sq=sq, ssq=ssq, pz=pz, pd=pd, pbk=pbk, pa=pa_ps, pS=pS, po=po, St=St, Sb=Sb, cm=cm,
                     wab=wab, negcol=negcol, o_d=o_d, og_d=og_d)
            for d in range(2):
                for h in range(4):
                    kb.memset("dve", St[h].v(), 0.0)
                    kb.memset("dve", Sb[h].v(), 0.0)
                ctx_blocks = list(range(TOWN // BT, S // BT))
                own_blocks = list(range(0, TOWN // BT))
                if d == 1:
                    ctx_blocks = ctx_blocks[::-1]
                    own_blocks = own_blocks[::-1]
                for (own, blocks) in ((False, ctx_blocks), (True, own_blocks)):
                    for bi in blocks:
                        p0 = bi * BT
                        bb = nblk % 2
                        nblk += 1
                        kB, vB = kblk[bb], vblk[bb]
                        kb.dma(kB.v(), k_d[p0:p0 + BT, :].rr("(c p) n -> p c n", p=64))
                        kb.dma(vB.v(), v_d[p0:p0 + BT, :].rr("(c p) n -> p c n", p=64))
                        aH = a16[bb]
                        kb.dma(aH[0:16, :], a1T_d[16 * d:16 * d + 16, p0:p0 + BT])
                        if own:
                            qB, ktB, oB, rB, ogB = qblk[bb], ktblk[bb], oblk[bb], rblk[bb], ogstg[bb]
                            kb.dma(qB.v(), qT_d[:, p0:p0 + BT].rr("(h p) t -> p h t", p=128))
                            kb.dma(ktB.v(), kT_d[:, p0:p0 + BT].rr("(h p) t -> p h t", p=128))
                            if d == 1:
                                kb.dma(oB.v(), o_d[p0:p0 + BT, :].rr("(c p) n -> p c n", p=64))
                                kb.dma(rB.v(), r_d[p0:p0 + BT, :].rr("(c p) n -> p c n", p=64))
                        js = list(range(NB)) if d == 0 else list(range(NB))[::-1]
                        if not own:
                            qB = ktB = oB = rB = ogB = None
                        for j in js:
                            c3 = nch % 3
                            nch += 1
                            last_ctx = (not own) and bi == blocks[-1] and j == js[-1]
                            blk_end = own and j == js[-1]
                            pipe.append(self._gla_chunk(kb, d, own, j, bi * NB + j, c3, last_ctx, blk_end, p0, BT,
                                                        dict(kB=kB, vB=vB, aH=aH, qB=qB, ktB=ktB, oB=oB, rB=rB, ogB=ogB), G))
                            n_ = len(pipe)
                            pipe[n_ - 1][0]()
                            if n_ >= 2:
                                pipe[n_ - 2][1]()
                            if n_ >= 3:
                                pipe[n_ - 3][2]()
                n_ = len(pipe)
                pipe[n_ - 1][1]()
                pipe[n_ - 2][2]()
                pipe[n_ - 1][2]()
                pipe = []

    def _gla_chunk(self, kb, d, own, j, chunk, c3, last_ctx, blk_end, p0, BT, B, G):
        tcol = lambda kind: self.tri[:, (d * 3 + kind) * 64:(d * 3 + kind) * 64 + 64]
        t16 = lambda kind: G["tri16"][:, (d * 3 + kind) * 64:(d * 3 + kind) * 64 + 64]
        sp_, sph, spl, kd_, ebl_, qt_, kt_ = G["sp"][c3], G["sp16"][c3], G["splo"][c3], G["kd"][c3], G["ebl"][c3], G["qtl"][c3], G["ktl"][c3]
        ez, ed, eb, enb, at, ot, sq, ssq = G["ez"], G["ed"], G["eb"], G["enb"], G["at"], G["ot"], G["sq"], G["ssq"]
        pz, pd, pbk, pa_, pS, po = G["pz"], G["pd"], G["pbk"], G["pa"], G["pS"], G["po"]
        St, Sb, cm = G["St"], G["Sb"], G["cm"]
        kB, vB, aH, qB, ktB, oB, rB, ogB = [B[k] for k in ("kB", "vB", "aH", "qB", "ktB", "oB", "rB", "ogB")]

        def A1():
            kb.mm(pz.v(), aH[:, j * 64:(j + 1) * 64], G["wab"][d].v())
            kb.act(ez.v(), pz.v(), AF.Exp, scale=-1.0)
            kb.act(sp_.v(), ez.v(), AF.Ln, bias=1.0)
            kb.act(sph.v(), ez.v(), AF.Ln, bias=1.0)
            kb.tt("dve", spl.v(), sp_.v(), sph.v(), ALU.subtract)

        def A2():
            kb.mm(pd.v(), t16(1), sph.v(), start=True, stop=False)
            kb.mm(pd.v(), t16(1), spl.v(), start=False, stop=True)
            if own:
                kb.act(ed.v(), pd.v(), AF.Exp)
            else:
                kb.act(ed.v(), pd.v(), AF.Exp, bias=cm[:, d * 256 + chunk:d * 256 + chunk + 1])
            kb.tt("dve", kd_.v(), kB[:, j, :], ed.v(), ALU.mult)
            if own:
                for h in range(4):
                    kb.mm(pbk[:, h * 64:(h + 1) * 64], sph[:, h * 128:(h + 1) * 128], t16(0), start=True, stop=False)
                    kb.mm(pbk[:, h * 64:(h + 1) * 64], spl[:, h * 128:(h + 1) * 128], t16(0), start=False, stop=True)
                pbv = pbk[:, 0:256].rr("p (h t) -> p h t", h=4)
                kb.act(eb.v(), pbv, AF.Exp)
                kb.act(enb.v(), pbv, AF.Exp, scale=-1.0)
                last = 63 if d == 0 else 0
                kb.cp("pool", ebl_.v(), eb[:, :, last])
                kb.stt("dve", qt_.v(), qB[:, :, j * 64:(j + 1) * 64], 128.0 ** -0.5, eb.v(), ALU.mult, ALU.mult)
                kb.tt("pool", kt_.v(), ktB[:, :, j * 64:(j + 1) * 64], enb.v(), ALU.mult)
            else:
                for h in range(4):
                    kb.mm(pbk[:, h:h + 1], sph[:, h * 128:(h + 1) * 128], G["negcol"].v())
                kb.act(ebl_.v(), pbk[:, 0:4], AF.Exp)

        def Bs():
            if own:
                pa = pa_[0:64, 0:256].rr("p (h t) -> p h t", h=4)
                for h in range(4):
                    kb.mm(pa[:, h, :], kt_[:, h, :], qt_[:, h, :])
                kb.tt("dve", at.v(), pa, tcol(2).us(1).bc([64, 4, 64]), ALU.mult)
                for h in range(4):
                    kb.mm(po[:, h, :], at[:, h, :], vB[:, j, h * 256:(h + 1) * 256], start=True, stop=False)
                    kb.mm(po[:, h, :], qt_[:, h, :], Sb[h].v(), start=False, stop=True)
                if d == 0:
                    kb.cp("act", oB[:, j, :], po.v().rr("p h e -> p (h e)"))
                else:
                    kb.tt("dve", ot.v(), po.v(), oB[:, j, :].rr("p (h e) -> p h e", h=4), ALU.add)
                    for h in range(4):
                        kb.act(sq[:, h, :], ot[:, h, :], AF.Square, accum=ssq[:, h:h + 1])
                    kb.ts("dve", ssq.v(), ssq.v(), 1.0 / 256.0, EPS, op0=ALU.mult, op1=ALU.add)
                    kb.act(ssq.v(), ssq.v(), AF.Sqrt)
                    kb.recip(ssq.v(), ssq.v())
                    for h in range(4):
                        kb.stt("dve", ot[:, h, :], ot[:, h, :], ssq[:, h:h + 1], self.gngb.v(), ALU.mult, ALU.mult)
                    kb.tt("dve", ogB[:, j, :], ot.v().rr("p h e -> p (h e)"), rB[:, j, :], ALU.mult)
            for h in range(4):
                kb.mm(pS[:, h, :], kd_[:, h * 128:(h + 1) * 128], vB[:, j, h * 256:(h + 1) * 256])
            for h in range(4):
                kb.stt("dve", St[h].v(), St[h].v(), ebl_[:, h:h + 1], pS[:, h, :], ALU.mult, ALU.add)
                if own or last_ctx:
                    kb.cp("act", Sb[h].v(), St[h].v())
            if blk_end:
                if d == 0:
                    kb.dma(G["o_d"][p0:p0 + BT, :].rr("(c p) n -> p c n", p=64), oB.v(), q="pool")
                else:
                    kb.dma(G["og_d"][p0:p0 + BT, :].rr("(c p) n -> p c n", p=64), ogB.v(), q="pool")

        return (A1, A2, Bs)

    def cvt_load(self, kb, ph, dst, src, n, stg):
        raise NotImplementedError

    def s_mix(self, kb, gp):
        self.s_mix_a(kb, gp)
        self.s_mix_b(kb, gp)
        self.s_mix_c(kb, gp)

    def load_w_bf16(self, kb, ph, dstbuf, src, nk, stg, n0=0):
        engs = ["dve", "pool", "act"]
        for kc in range(nk):
            st = stg[(n0 + kc) % len(stg)]
            kb.dma(st.v(), src[kc * 128:(kc + 1) * 128, :])
            kb.cp(engs[kc % 3], dstbuf[:, kc, :], st.v())

    def s_mix_a(self, kb, gp):
        I = self.inp
        UT_d, og_d, m_d = self.scr["UT_d"], self.scr["og_d"], self.scr["m_d"]
        mT_d = self.scratch("mT_d", [D, TOWN], BF16)
        with Phase(kb, "mixa") as ph:
            wgl = ph.sb([128, 8, D], BF16)
            wfp = ph.sb([128, 16, D], BF16)
            with Phase(kb, "mixa0") as ph0:
                stg = [ph0.sb([128, D], F32) for _ in range(3)]
                wfn = ph0.sb([128, 8, D], BF16)
                self.load_w_bf16(kb, ph0, wfn, I["w_fnet"], 8, stg)
                self.load_w_bf16(kb, ph0, wgl, I["w_gla"], 8, stg)
                cct = ph0.sb([128, 2, 512], BF16)
                kb.dma(cct.v(), I["cc"].v().rr("(h p) n -> p h n", p=128))
                pw = [ph0.ps([128, 512], F32) for _ in range(2)]
                nn = 0
                for g in range(4):
                    for c in range(2):
                        for hh in range(2):
                            for cc in range(4):
                                p = pw[nn % 2]
                                nn += 1
                                for half in range(2):
                                    kb.mm(p.v(), cct[:, half, c * 256 + hh * 128:c * 256 + hh * 128 + 128],
                                          wfn[:, g * 2 + half, cc * 512:(cc + 1) * 512], start=(half == 0), stop=(half == 1))
                                kb.cp("act" if nn % 2 == 0 else "dve", wfp[:, (g * 2 + hh) * 2 + c, cc * 512:(cc + 1) * 512], p.v())
            utT = [ph.sb([128, 8, 2, 128], BF16) for _ in range(2)]
            ogt = [ph.sb([128, 1024], BF16) for _ in range(2)]
            sgt = [ph.sb([128, 4096], BF16) for _ in range(2)]
            ogT = ph.sb([128, 8, 128], BF16)
            ta = ph.sb([128, 1024], F32)
            tb = ph.sb([128, 1024], F32)
            mg = ph.sb([128, D], BF16)
            mstg = [ph.sb([128, 16, 512], BF16) for _ in range(2)]
            pyf = ph.ps([128, 1024], F32)
            pyg = ph.ps([128, 1024], F32)
            pog = ph.ps([128, 1024], BF16)
            pmt = ph.ps([128, 2048], BF16)
            for tl in range(32):
                t0 = tl * 128
                u_, o_, s_ = utT[tl % 2], ogt[tl % 2], sgt[tl % 2]
                for c_ in range(2):
                    kb.dma(u_[:, :, c_, :], UT_d[:, :, c_, t0:t0 + 128].rr("b p t -> p b t"))
                kb.dma(o_.v(), og_d[t0:t0 + 128, :])
                kb.dma(s_.v(), m_d[t0:t0 + 128, :])
                for kc in range(8):
                    kb.tr(pog[:, kc * 128:(kc + 1) * 128], o_[:, kc * 128:(kc + 1) * 128], self.identb.v())
                kb.cp("act", ogT.v().rr("p k t -> p (k t)"), pog.v())
                for hf in range(2):
                    for cc in range(2):
                        c0 = hf * 1024 + cc * 512
                        for kk in range(16):
                            kb.mm(pyf[:, cc * 512:(cc + 1) * 512], u_[:, kk // 2, kk % 2, :], wfp[:, kk, c0:c0 + 512],
                                  start=(kk == 0), stop=(kk == 15))
                        for kk in range(8):
                            kb.mm(pyg[:, cc * 512:(cc + 1) * 512], ogT[:, kk, :], wgl[:, kk, c0:c0 + 512],
                                  start=(kk == 0), stop=(kk == 7))
                    kb.tt("dve", ta.v(), pyf.v(), s_[:, hf * 1024:(hf + 1) * 1024], ALU.mult)
                    kb.tt("dve", tb.v(), pyg.v(), s_[:, 2048 + hf * 1024:2048 + (hf + 1) * 1024], ALU.mult)
                    kb.tt("dve", mg[:, hf * 1024:(hf + 1) * 1024], ta.v(), tb.v(), ALU.add)
                for kc in range(16):
                    kb.tr(pmt[:, kc * 128:(kc + 1) * 128], mg[:, kc * 128:(kc + 1) * 128], self.identb.v())
                ms = mstg[(tl // 4) % 2]
                kb.cp("act", ms[:, :, (tl % 4) * 128:(tl % 4) * 128 + 128], pmt.v().rr("p (k t) -> p k t", k=16))
                if tl % 4 == 3:
                    tt0 = (tl // 4) * 512
                    kb.dma(mT_d[:, tt0:tt0 + 512].rr("(k p) t -> p k t", p=128), ms.v(), q="pool")

    def s_mix_b(self, kb, gp):
        I = self.inp
        mT_d = self.scr["mT_d"]
        h_d = self.scratch("h_d", [TOWN, D], F32)
        n2T_d = self.scratch("n2T_d", [D, TOWN], BF16)
        with Phase(kb, "mixb") as ph:
            wo = ph.sb([128, 16, D], BF16)
            with Phase(kb, "mixb0") as ph0:
                stg = [ph0.sb([128, D], F32) for _ in range(2)]
                self.load_w_bf16(kb, ph0, wo, I["w_out"], 16, stg)
            mT = [ph.sb([128, 16, 512], BF16) for _ in range(1)]
            ht = ph.sb([128, 4, D], F32)
            xt = ht
            tmp = ph.sb([128, D], F32)
            xb = ph.sb([128, 4, D], BF16)
            junk = tmp
            ss = ph.sb([128, 4], F32)
            rstd = ph.sb([128, 4], F32)
            nT = ph.sb([128, 16, 512], BF16)
            pmix = ph.ps([128, D], F32)
            pst = [ph.ps([128, 1024], BF16) for _ in range(2)]
            g1b = ph.sb([128, D], F32)
            self.g1b = g1b
            with Phase(kb, "mixb1") as ph1:
                rep = ph1.sb([128, 16, 128], F32)
                kb.cp("dve", rep.v(), self.g1.us(2).bc([128, 16, 128]))
                for kc in range(16):
                    kb.mm(pmix[:, kc * 128:(kc + 1) * 128], rep[:, kc, :], self.identf.v())
                kb.cp("act", g1b.v(), pmix.v())
            for tl in range(8):
                t0 = tl * 512
                m_ = mT[0]
                kb.dma(m_.v(), mT_d[:, t0:t0 + 512].rr("(k p) t -> p k t", p=128))
                kb.dma(xt.v(), I["x_own"][t0:t0 + 512, :].rr("(s p) d -> p s d", p=128))
                for s in range(4):
                    for cc in range(4):
                        for kc in range(16):
                            kb.mm(pmix[:, cc * 512:(cc + 1) * 512], m_[:, kc, s * 128:(s + 1) * 128], wo[:, kc, cc * 512:(cc + 1) * 512],
                                  start=(kc == 0), stop=(kc == 15))
                    kb.tt("dve", tmp.v(), pmix.v(), self.g1b.v(), ALU.mult)
                    kb.tt("dve", ht[:, s, :], tmp.v(), xt[:, s, :], ALU.add)
                kb.dma(h_d[t0:t0 + 512, :].rr("(s p) d -> p s d", p=128), ht.v(), q="pool")
                self.norm_transpose(kb, ph, ht, 4, self.gam2, self.sh2, nT, pst, xb, ss, rstd, junk)
                kb.dma(n2T_d[:, t0:t0 + 512].rr("(k p) t -> p k t", p=128), nT.v(), q="pool")

    def s_mix_c(self, kb, gp):
        I = self.inp
        n2T_d = self.scr["n2T_d"]
        eT_d = self.scratch("eT_d", [3, 128, TOWN], F32)
        with Phase(kb, "mixc") as ph:
            wq = ph.sb([128, 16, D], BF16)
            keysT = ph.sb([128, 16, 128], BF16)
            with Phase(kb, "mixc0") as ph0:
                stg = [ph0.sb([128, D], F32) for _ in range(2)]
                self.load_w_bf16(kb, ph0, wq, I["peer_w_q"], 16, stg)
                kf = ph0.sb([128, D], F32)
                kb.dma(kf.v().rr("p (a d) -> p a d", a=16), I["peer_keys"].v().rr("a k d -> k a d"))
                kbf = ph0.sb([128, 16, 128], BF16)
                kb.cp("dve", kbf.v().rr("p a d -> p (a d)"), kf.v())
                pkt = ph0.ps([128, 2048], BF16)
                for a in range(16):
                    kb.tr(pkt[:, a * 128:(a + 1) * 128], kbf[:, a, :], self.identb.v())
                kb.cp("act", keysT.v().rr("p a k -> p (a k)"), pkt.v())
            n2T = ph.sb([128, 16, 512], BF16)
            qTs = [ph.sb([128, 16, 512], BF16) for _ in range(2)]
            scs = [ph.sb([128, 16, 128], F32) for _ in range(2)]
            tmpm = ph.sb([128, 16, 128], F32)
            v16 = ph.sb([128, 16, 16], F32)
            i16 = ph.sb([128, 16, 16], U32)
            idxf = ph.sb([128, 16, 16], F32)
            cand = ph.sb([128, 8, 256], F32)
            cand2 = Buf(tmpm.ap.rearrange("p a k -> p (a k)").rearrange("p (h c) -> p h c", h=8))
            sv = ph.sb([128, 8, 16], F32)
            pos = ph.sb([128, 8, 16], U32)
            pq = ph.sb([128, 2, 128], U32)
            pqf = ph.sb([128, 2, 128], F32)
            oh = ph.sb([128, 128, 16], F32)
            e12 = ph.sb([128, 2, 128], F32)
            gex = ph.sb([128, 8, 16], F32)
            gz = ph.sb([128, 8], F32)
            gg = ph.sb([128, 128], F32)
            estg = [ph.sb([128, 3, 512], F32) for _ in range(1)]
            v16s = [Buf(v16.ap[:, cc, :]) for cc in range(16)]
            i16s = [Buf(i16.ap[:, cc, :]) for cc in range(16)]
            tmps = [Buf(tmpm.ap[:, cc, :]) for cc in range(16)]
            svs = [Buf(sv.ap[:, h, :]) for h in range(8)]
            poss = [Buf(pos.ap[:, h, :]) for h in range(8)]
            c2s = [Buf(cand2.ap[:, h, :]) for h in range(8)]
            pq_ = [ph.ps([128, 512], F32) for _ in range(2)]
            psc = ph.ps([128, 2048], F32)
            ptr = ph.ps([128, 512], F32)
            dv = kb.eng["dve"]
            iota16 = self.iota[:, 0:16]
            def emit_qT(tl, part):
                t0 = tl * 512
                if part == 0:
                    kb.dma(n2T.v(), n2T_d[:, t0:t0 + 512].rr("(k p) t -> p k t", p=128))
                q_ = qTs[tl % 2]
                for cc in range(part * 4, part * 4 + 4):
                    p = pq_[cc % 2]
                    for kc in range(16):
                        kb.mm(p.v(), wq[:, kc, cc * 128:(cc + 1) * 128], n2T[:, kc, :], start=(kc == 0), stop=(kc == 15))
                    kb.cp("act", q_[:, cc, :], p.v())

            def emit_scores(tl, s):
                q_ = qTs[tl % 2]
                sc = scs[(tl * 4 + s) % 2]
                for cc in range(16):
                    kb.mm(psc[:, cc * 128:(cc + 1) * 128], q_[:, cc, s * 128:(s + 1) * 128], keysT[:, cc, :])
                kb.cp("act", sc.v().rr("p a k -> p (a k)"), psc.v())

            def emit_chain_tail(tl, s):
                t0 = tl * 512
                sc = scs[(tl * 4 + s) % 2]
                es_ = estg[0]
                for cc in range(16):
                    kb.op("dve", [sc.v()], [v16s[cc].v()], lambda g, cc=cc: g.max(out=v16[:, cc, 0:8].ap, in_=sc[:, cc, :].ap))
                for cc in range(16):
                    kb.op("dve", [sc.v(), v16s[cc].v()], [i16s[cc].v()], lambda g, cc=cc: g.max_index(out=i16[:, cc, 0:8].ap, in_max=v16[:, cc, 0:8].ap, in_values=sc[:, cc, :].ap))
                for cc in range(16):
                    kb.op("dve", [sc.v(), v16s[cc].v()], [tmps[cc].v()], lambda g, cc=cc: g.match_replace(out=tmpm[:, cc, :].ap, in_to_replace=v16[:, cc, 0:8].ap, in_values=sc[:, cc, :].ap, imm_value=-1e30))
                for cc in range(16):
                    kb.op("dve", [tmps[cc].v()], [v16s[cc].v()], lambda g, cc=cc: g.max(out=v16[:, cc, 8:16].ap, in_=tmpm[:, cc, :].ap))
                for cc in range(16):
                    kb.op("dve", [tmps[cc].v(), v16s[cc].v()], [i16s[cc].v()], lambda g, cc=cc: g.max_index(out=i16[:, cc, 8:16].ap, in_max=v16[:, cc, 8:16].ap, in_values=tmpm[:, cc, :].ap))
                kb.cp("dve", idxf.v(), V(i16, i16.ap, i16s))
                v4 = V(v16, v16.ap, v16s).rr("p (h f) k -> p h f k", f=2)
                kb.tt("dve", cand.v().rr("p h (a b) -> p h a b", a=16), v4[:, :, 0, :].us(3).bc([128, 8, 16, 16]),
                      v4[:, :, 1, :].us(2).bc([128, 8, 16, 16]), ALU.add)
                for h in range(8):
                    kb.op("dve", [cand.v()], [svs[h].v()], lambda g, h=h: g.max(out=sv[:, h, 0:8].ap, in_=cand[:, h, :].ap))
                for h in range(8):
                    kb.op("dve", [cand.v(), svs[h].v()], [poss[h].v()], lambda g, h=h: g.max_index(out=pos[:, h, 0:8].ap, in_max=sv[:, h, 0:8].ap, in_values=cand[:, h, :].ap))
                for h in range(8):
                    kb.op("dve", [cand.v(), svs[h].v()], [c2s[h].v()], lambda g, h=h: g.match_replace(out=cand2[:, h, :].ap, in_to_replace=sv[:, h, 0:8].ap, in_values=cand[:, h, :].ap, imm_value=-1e30))
                for h in range(8):
                    kb.op("dve", [c2s[h].v()], [svs[h].v()], lambda g, h=h: g.max(out=sv[:, h, 8:16].ap, in_=cand2[:, h, :].ap))
                for h in range(8):
                    kb.op("dve", [c2s[h].v(), svs[h].v()], [poss[h].v()], lambda g, h=h: g.max_index(out=pos[:, h, 8:16].ap, in_max=sv[:, h, 8:16].ap, in_values=cand2[:, h, :].ap))
                posv = pos.v().rr("p h k -> p (h k)")
                posw = V(pos, pos.ap, poss)
                svw = V(sv, sv.ap, svs)
                kb.op("dve", [posw], [pq.v()], lambda g: g.tensor_single_scalar(out=pq[:, 0, :].ap, in_=posv.ap, scalar=4, op=ALU.logical_shift_right))
                kb.op("dve", [posw], [pq.v()], lambda g: g.tensor_single_scalar(out=pq[:, 1, :].ap, in_=posv.ap, scalar=15, op=ALU.bitwise_and))
                kb.cp("dve", pqf.v(), pq.v())
                i4 = idxf.v().rr("p (h f) k -> p h f k", f=2)
                for f_ in range(2):
                    kb.tt("dve", oh.v(), pqf[:, f_, :].us(2).bc([128, 128, 16]), iota16.us(1).bc([128, 128, 16]), ALU.is_equal)
                    kb.tt("dve", oh.v().rr("p (h k) a -> p h k a", h=8), oh.v().rr("p (h k) a -> p h k a", h=8),
                          i4[:, :, f_, :].us(2).bc([128, 8, 16, 16]), ALU.mult)
                    kb.op("dve", [oh.v()], [e12.v()], lambda g, f_=f_: g.tensor_reduce(out=e12[:, f_, :].ap, in_=oh.v().ap, axis=AX.X, op=ALU.add))
                kb.tt("dve", gex.v(), svw, svw[:, :, 0:1].bc([128, 8, 16]), ALU.subtract)
                kb.act(gex.v(), gex.v(), AF.Exp)
                kb.op("dve", [gex.v()], [gz.v()], lambda g: g.tensor_reduce(out=gz.v().ap, in_=gex.v().ap, axis=AX.X, op=ALU.add))
                kb.recip(gz.v(), gz.v())
                kb.tt("dve", gg.v().rr("p (h k) -> p h k", h=8), gex.v(), gz.v().us(2).bc([128, 8, 16]), ALU.mult)
                for j, src in enumerate((e12[:, 0, :], e12[:, 1, :], gg.v())):
                    kb.mm(ptr[:, j * 128:(j + 1) * 128], src, self.identf.v())
                kb.cp("act", es_[:, :, s * 128:(s + 1) * 128], ptr[:, 0:384].rr("p (j t) -> p j t", j=3))
                if s == 3:
                    kb.dma(eT_d[:, :, t0:t0 + 512].rr("j p t -> p j t"), es_.v(), q="pool")

            for part in range(4):
                emit_qT(0, part)
            emit_scores(0, 0)
            for tl in range(8):
                for s in range(4):
                    if tl + 1 < 8:
                        emit_qT(tl + 1, s)
                    if s < 3:
                        emit_scores(tl, s + 1)
                    elif tl + 1 < 8:
                        emit_scores(tl + 1, 0)
                    emit_chain_tail(tl, s)

    def s_peer_prep(self, kb, gp):
        I = self.inp
        uT_d = self.scratch("uT_d", [128, 128, 16, 128], BF16)
        vb_d = self.scratch("vb_d", [16384, D], BF16)
        with Phase(kb, "pprep") as ph:
            ust = [ph.sb([128, D], F32) for _ in range(2)]
            vst = [ph.sb([128, D], F32) for _ in range(2)]
            ub = [ph.sb([128, D], BF16) for _ in range(2)]
            vb = [ph.sb([128, 4, D], BF16) for _ in range(2)]
            uo = [ph.sb([128, 4, 16, 128], BF16) for _ in range(2)]
            pt = [ph.ps([128, 2048], BF16) for _ in range(2)]
            for c in range(128):
                g, ci = c // 4, c % 4
                u_, v_ = ust[c % 2], vst[c % 2]
                kb.dma(u_.v(), I["peer_u"][c * 128:(c + 1) * 128, :])
                kb.dma(v_.v(), I["peer_v"][c * 128:(c + 1) * 128, :])
                kb.cp("dve", ub[c % 2].v(), u_.v())
                kb.cp("pool", vb[g % 2][:, ci, :], v_.v())
                p = pt[c % 2]
                for kc in range(16):
                    kb.tr(p[:, kc * 128:(kc + 1) * 128], ub[c % 2][:, kc * 128:(kc + 1) * 128], self.identb.v())
                kb.cp("act", uo[g % 2][:, ci, :, :].rr("p k e -> p (k e)"), p.v())
                if ci == 3:
                    kb.dma(uT_d[g * 4:(g + 1) * 4].rr("c p k e -> p c (k e)"), uo[g % 2].v().rr("p c k e -> p c (k e)"), q="pool")
                    kb.dma(vb_d[g * 512:(g + 1) * 512, :].rr("(c p) n -> p c n", p=128), vb[g % 2].v(), q="pool")

    def s_peer(self, kb, gp):
        I = self.inp
        uT_d, vb_d, n2T_d, eT_d, h_d = [self.scr[k] for k in ("uT_d", "vb_d", "n2T_d", "eT_d", "h_d")]
        TT = 256
        with Phase(kb, "peer") as ph:
            n2T = ph.sb([128, 16, TT], BF16)
            eT = ph.sb([128, 3, TT], F32)
            eTb = ph.sb([128, 2, TT], BF16)
            iob = ph.sb([128, 128], BF16)
            kb.cp("dve", iob.v(), self.iota.v())
            AC = ph.sb([128, 128, TT], BF16)
            Lbs = [ph.sb([128, 16, 128], BF16) for _ in range(2)]
            REs = [ph.sb([128, 2, 16, 128], BF16) for _ in range(2)]
            us = [ph.sb([128, 2, 16, 128], BF16) for _ in range(4)]
            vs = [ph.sb([128, 4, 1024], BF16) for _ in range(3)]
            hout = ph.sb([128, 2, D], F32)
            tmp = ph.sb([128, D], F32)
            ss = ph.sb([128, 2], F32)
            pu = [ph.ps([128, 512], F32) for _ in range(2)]
            pvs = [ph.ps([128, 1024], F32) for _ in range(2)]
            pg = [ph.ps([128, 512], F32) for _ in range(2)]
            ACh = [Buf(AC.ap[:, 0:64, :]), Buf(AC.ap[:, 64:128, :])]
            nu = nv = ng = 0
            for tl in range(TOWN // TT):
                t0 = tl * TT
                kb.dma(n2T.v(), n2T_d[:, t0:t0 + TT].rr("(k p) t -> p k t", p=128))
                kb.dma(eT.v(), eT_d[:, :, t0:t0 + TT].rr("j p t -> p j t"))
                kb.dma(hout.v(), h_d[t0:t0 + TT, :].rr("(s p) d -> p s d", p=128))
                def build_lr(seq):
                    Lb_, RE_ = Lbs[seq % 2], REs[seq % 2]
                    a0 = (seq % 16) * 16
                    io = iob.v().us(1).us(1).bc([128, 2, 16, 128])
                    kb.tt("dve", RE_.v(), eTb[:, :, a0:a0 + 16].us(3).bc([128, 2, 16, 128]), io, ALU.is_equal)
                    kb.tt("pool", Lb_.v(), RE_[:, 0, :, :], eT[:, 2, a0:a0 + 16].us(2).bc([128, 16, 128]), ALU.mult)

                def g_sub(seq):
                    nonlocal ng
                    half, st = seq // 16, seq % 16
                    if seq + 1 < 32:
                        build_lr(seq + 1)
                    Lb, Rb = Lbs[seq % 2], REs[seq % 2][:, 1, :, :]
                    ach = ACh[half]
                    for tg in range(2):
                        p = pg[ng % 2]
                        ng += 1
                        pgv = p.v().rr("p (t c) -> p c t", t=8)
                        for t in range(8):
                            tt_ = tg * 8 + t
                            kb.mm(p[:, t * 64:(t + 1) * 64], Rb[:, tt_, :], Lb[:, tt_, half * 64:(half + 1) * 64])
                        tk = st * 16 + tg * 8
                        acv = ach[:, :, tk:tk + 8]
                        kb.tt("dve", acv, acv, pgv, ALU.mult)

                def u_chunks(cg):
                    nonlocal nu
                    u_ = us[nu % 4]
                    nu += 1
                    kb.dma(u_.v().rr("p c k e -> p c (k e)"), uT_d[cg * 2:(cg + 1) * 2].rr("c p k e -> p c (k e)"))
                    for ci in range(2):
                        c = cg * 2 + ci
                        p = pu[c % 2]
                        for kc in range(16):
                            kb.mm(p[:, 0:TT], u_[:, ci, kc, :], n2T[:, kc, :], start=(kc == 0), stop=(kc == 15))
                        kb.act(ACh[c // 64][:, c % 64, :], p[:, 0:TT], AF.Gelu)

                kb.cp("act", eTb.v(), eT[:, 0:2, :])
                build_lr(0)
                for cg in range(32):
                    u_chunks(cg)
                for cg in range(32, 64):
                    u_chunks(cg)
                    if cg % 2 == 1:
                        g_sub((cg - 32) // 2)
                for half in range(2):
                    for cg in range(32):
                        v_ = vs[nv % 3]
                        nv += 1
                        kb.dma(v_.v(), vb_d[cg * 512:(cg + 1) * 512, half * 1024:(half + 1) * 1024].rr("(c p) n -> p c n", p=128))
                        if half == 0 and cg < 16:
                            g_sub(16 + cg)
                        for ci in range(4):
                            c = cg * 4 + ci
                            for ts in range(2):
                                for cc in range(2):
                                    kb.mm(pvs[ts][:, cc * 512:(cc + 1) * 512], ACh[c // 64][:, c % 64, ts * 128:(ts + 1) * 128], v_[:, ci, cc * 512:(cc + 1) * 512],
                                          start=(c == 0), stop=(c == 127))
                    for ts in range(2):
                        hc = slice(half * 1024, (half + 1) * 1024)
                        kb.tt("dve", tmp[:, 0:1024], pvs[ts].v(), self.g2b[:, hc], ALU.mult)
                        kb.tt("dve", hout[:, ts, hc], tmp[:, 0:1024], hout[:, ts, hc], ALU.add)
                for ts in range(2):
                    kb.act(tmp.v(), hout[:, ts, :], AF.Square, accum=ss[:, ts:ts + 1])
                kb.ts("dve", ss.v(), ss.v(), 1.0 / D, EPS, op0=ALU.mult, op1=ALU.add)
                kb.act(ss.v(), ss.v(), AF.Sqrt)
                kb.recip(ss.v(), ss.v())
                for ts in range(2):
                    kb.stt("dve", hout[:, ts, :], hout[:, ts, :], ss[:, ts:ts + 1], self.fngb.v(), ALU.mult, ALU.mult)
                kb.dma(self.out[t0:t0 + TT, :].rr("(s p) d -> p s d", p=128), hout.v(), q="pool")


def make_in_maps(inputs):
    x = np.asarray(inputs["x"], dtype=np.float32)
    sq = lambda a: np.ascontiguousarray(np.asarray(a, dtype=np.float32))
    shared = {
        "w_ada": sq(inputs["w_ada"][0]), "b_ada": sq(inputs["b_ada"]).reshape(1, -1),
        "norm1_g": sq(inputs["norm1_g"]).reshape(1, -1), "w_in": sq(inputs["w_in"][0]),
        "w_fnet": sq(inputs["w_fnet"][0]), "gla_w_a2": sq(inputs["gla_w_a2"][0]), "gla_b_a": sq(inputs["gla_b_a"][0]),
        "gla_norm_g": sq(inputs["gla_norm_g"]).reshape(1, -1), "w_gla": sq(inputs["w_gla"][0]),
        "w_out": sq(inputs["w_out"][0]), "norm2_g": sq(inputs["norm2_g"]).reshape(1, -1),
        "peer_w_q": sq(inputs["peer_w_q"][0]), "peer_keys": sq(inputs["peer_keys"][0]).reshape(16, 128, 128),
        "peer_u": sq(inputs["peer_u"][0]), "peer_v": sq(inputs["peer_v"][0]),
        "final_norm_g": sq(inputs["final_norm_g"]).reshape(1, -1),
    }
    consts = [host_consts(r) for r in range(4)]
    maps = []
    for c in range(NCORES):
        b, r = c // 4, c % 4
        T0 = TOWN * r
        m = dict(shared)
        m["xs"] = np.ascontiguousarray(np.roll(x[b], -T0, axis=0))
        m["x_own"] = np.ascontiguousarray(x[b, T0:T0 + TOWN])
        m["c"] = sq(inputs["c"][b:b + 1])
        m.update(consts[r])
        maps.append(m)
    return maps


def run_prog(inputs, stop_after=None, debug=(), cores=NCORES):
    p = Prog(stop_after=stop_after, debug=debug)
    nc = p.build()
    maps = make_in_maps(inputs)[:cores]
    res = run_bass_kernel_spmd(nc, maps, core_ids=list(range(cores)))
    return p, res


def kernel(**inputs):
    p, res = run_prog(inputs)
    x = np.asarray(inputs["x"])
    out = np.empty(x.shape, dtype=np.float32)
    for c in range(NCORES):
        b, r = c // 4, c % 4
        out[b, TOWN * r:TOWN * (r + 1)] = res.results[c]["out"]
    return out
```
